# Optimizing a Trainium2 kernel written in Bass

```python
import jax, jax.numpy as jnp
from jax import lax
import numpy as np

D_MODEL = 2048
BATCH = 8
SEQ = 2048
DEPTH = 1

EPS = 1e-6

POOL_WINDOWS = (2, 4, 8, 16)
POOL_GROUPS = len(POOL_WINDOWS)
POOL_WIDTH = D_MODEL // 2
POOL_GROUP_DIM = POOL_WIDTH // POOL_GROUPS
POOL_OUT_DIM = D_MODEL // POOL_GROUPS

HG_EXPAND = 128
HG_HEADS = D_MODEL // HG_EXPAND
HG_HEAD_V = D_MODEL // HG_HEADS
HG_KEY_WIDTH = HG_HEADS * HG_EXPAND
HG_VAL_WIDTH = HG_HEADS * HG_HEAD_V
HG_CHUNK = 64

IN_SPLIT_WIDTHS = (POOL_WIDTH, HG_KEY_WIDTH, HG_KEY_WIDTH, HG_VAL_WIDTH, HG_VAL_WIDTH, D_MODEL, D_MODEL)
IN_WIDTH = sum(IN_SPLIT_WIDTHS)

PEER_HEADS = 8
PEER_NKEYS = 128
PEER_EXPERTS = PEER_NKEYS * PEER_NKEYS
PEER_DKEY = 256
PEER_TOPK = 16
PEER_BLOCK = 128

kernel_name = "hybrid_pool_hgrn2_peer_adaln"


def rms_norm(x, gain):
    xf = x.astype(jnp.float32)
    y = xf * lax.rsqrt(jnp.mean(xf * xf, axis=-1, keepdims=True) + EPS)
    return (y * gain.astype(jnp.float32)).astype(x.dtype)


def modulate(h, shift, scale):
    return h * (1.0 + scale[:, None, :]) + shift[:, None, :]


def pool_mixer(p, pool_w, pool_scale):
    b, s, _ = p.shape
    pf = p.astype(jnp.float32)
    cs = jnp.cumsum(pf, axis=1)
    pos = jnp.arange(s)
    outs = []
    for g, w in enumerate(POOL_WINDOWS):
        lo, hi = g * POOL_GROUP_DIM, (g + 1) * POOL_GROUP_DIM
        csg = cs[..., lo:hi]
        prev = jnp.pad(csg, ((0, 0), (w, 0), (0, 0)))[:, :s]
        cnt = jnp.minimum(pos + 1, w).astype(jnp.float32)[None, :, None]
        outs.append((csg - prev) / cnt - pf[..., lo:hi])
    pooled = jnp.stack(outs, axis=2)
    y = jnp.einsum('bsgd,gde->bsge', pooled, pool_w.astype(jnp.float32))
    y = y.reshape(b, s, D_MODEL) * pool_scale.astype(jnp.float32)
    return y.astype(p.dtype)


def hgrn2_mixer(q, f_logit, i, g, lb, hg_norm):
    b, s, _ = q.shape
    n = s // HG_CHUNK
    lbf = lb.astype(jnp.float32)
    log_f = jnp.logaddexp(jnp.log(lbf), jnp.log1p(-lbf) + jax.nn.log_sigmoid(f_logit.astype(jnp.float32)))
    k = -jnp.expm1(log_f)

    def to_chunks(t, d):
        return t.astype(jnp.float32).reshape(b, n, HG_CHUNK, HG_HEADS, d).transpose(1, 0, 3, 2, 4)

    qc = to_chunks(q, HG_EXPAND)
    kc = to_chunks(k, HG_EXPAND)
    lfc = to_chunks(log_f, HG_EXPAND)
    ic = to_chunks(i, HG_HEAD_V)
    mask = jnp.tril(jnp.ones((HG_CHUNK, HG_CHUNK), dtype=bool))[:, :, None]

    def step(state, inp):
        qx, kx, lfx, ix = inp
        cum = jnp.cumsum(lfx, axis=2)
        diff = cum[:, :, :, None, :] - cum[:, :, None, :, :]
        decay = jnp.exp(jnp.where(mask, diff, -jnp.inf))
        attn = jnp.einsum('bhtk,bhsk,bhtsk->bhts', qx, kx, decay)
        o = jnp.einsum('bhts,bhsv->bhtv', attn, ix) + jnp.einsum('bhtk,bhkv->bhtv', qx * jnp.exp(cum), state)
        last = cum[:, :, -1:, :]
        new_state = jnp.exp(last[:, :, 0, :])[..., None] * state + jnp.einsum('bhsk,bhsv->bhkv', kx * jnp.exp(last - cum), ix)
        return new_state, o

    state0 = jnp.zeros((b, HG_HEADS, HG_EXPAND, HG_HEAD_V), jnp.float32)
    _, o = lax.scan(step, state0, (qc, kc, lfc, ic))
    o = o.transpose(1, 0, 3, 2, 4).reshape(b, s, HG_HEADS, HG_HEAD_V)
    o = o * lax.rsqrt(jnp.mean(o * o, axis=-1, keepdims=True) + EPS) * hg_norm.astype(jnp.float32)
    o = o.reshape(b, s, HG_VAL_WIDTH) * jax.nn.silu(g.astype(jnp.float32))
    return o.astype(q.dtype)


def peer_ffn(h, w_q, sub_keys, u, v):
    b, s, d = h.shape
    t = b * s
    hf = h.reshape(t, d)
    q = (hf @ w_q).astype(jnp.float32).reshape(t, PEER_HEADS, 2, PEER_DKEY // 2)
    scores = jnp.einsum('thpd,hpnd->thpn', q, sub_keys.astype(jnp.float32))
    vals, idx = lax.top_k(scores, PEER_TOPK)
    cand = (vals[:, :, 0, :, None] + vals[:, :, 1, None, :]).reshape(t, PEER_HEADS, PEER_TOPK * PEER_TOPK)
    cand_idx = (idx[:, :, 0, :, None] * PEER_NKEYS + idx[:, :, 1, None, :]).reshape(t, PEER_HEADS, PEER_TOPK * PEER_TOPK)
    top_s, sel = lax.top_k(cand, PEER_TOPK)
    experts = jnp.take_along_axis(cand_idx, sel, axis=-1)
    gates = jax.nn.softmax(top_s, axis=-1)
    nb = t // PEER_BLOCK
    xb = hf.reshape(nb, PEER_BLOCK, d)
    eb = experts.reshape(nb, PEER_BLOCK, PEER_HEADS * PEER_TOPK)
    gb = gates.reshape(nb, PEER_BLOCK, PEER_HEADS * PEER_TOPK)

    def block_fn(args):
        xk, ek, gk = args
        hid = jax.nn.gelu(jnp.einsum('td,ted->te', xk, u[ek]).astype(jnp.float32), approximate=False)
        return jnp.einsum('te,ted->td', (gk * hid).astype(v.dtype), v[ek])

    y = lax.map(block_fn, (xb, eb, gb))
    return y.reshape(b, s, d).astype(h.dtype)


def setup_inputs(seed: int = 0) -> dict:
    key = jax.random.key(seed)
    ks = jax.random.split(key, 20)
    f32 = jnp.float32
    nrm = lambda k, shape, sc: jax.random.normal(k, shape, f32) * sc
    return {
        "x": nrm(ks[0], (BATCH, SEQ, D_MODEL), 1.0),
        "c": nrm(ks[1], (BATCH, D_MODEL), 1.0),
        "w_ada": nrm(ks[2], (DEPTH, D_MODEL, 6 * D_MODEL), 0.1 * D_MODEL ** -0.5),
        "b_ada": nrm(ks[3], (DEPTH, 6 * D_MODEL), 0.01),
        "norm1": 1.0 + nrm(ks[4], (DEPTH, D_MODEL), 0.02),
        "w_in": nrm(ks[5], (DEPTH, D_MODEL, IN_WIDTH), D_MODEL ** -0.5),
        "pool_w": nrm(ks[6], (DEPTH, POOL_GROUPS, POOL_GROUP_DIM, POOL_OUT_DIM), POOL_GROUP_DIM ** -0.5),
        "pool_scale": 1.0 + nrm(ks[7], (DEPTH, D_MODEL), 0.02),
        "lb_logits": nrm(ks[8], (DEPTH + 1, HG_KEY_WIDTH), 0.5),
        "hg_norm": 1.0 + nrm(ks[9], (DEPTH, HG_HEADS, HG_HEAD_V), 0.02),
        "w_out": nrm(ks[10], (DEPTH, D_MODEL, D_MODEL), D_MODEL ** -0.5),
        "norm2": 1.0 + nrm(ks[11], (DEPTH, D_MODEL), 0.02),
        "peer_wq": nrm(ks[12], (DEPTH, D_MODEL, PEER_HEADS * PEER_DKEY), D_MODEL ** -0.5),
        "peer_keys": nrm(ks[13], (DEPTH, PEER_HEADS, 2, PEER_NKEYS, PEER_DKEY // 2), (PEER_DKEY // 2) ** -0.5),
        "peer_u": nrm(ks[14], (DEPTH, PEER_EXPERTS, D_MODEL), D_MODEL ** -0.5),
        "peer_v": nrm(ks[15], (DEPTH, PEER_EXPERTS, D_MODEL), PEER_HEADS ** -0.5),
        "final_norm": 1.0 + nrm(ks[16], (D_MODEL,), 0.02),
    }


def reference(x, c, w_ada, b_ada, norm1, w_in, pool_w, pool_scale, lb_logits, hg_norm, w_out,
              norm2, peer_wq, peer_keys, peer_u, peer_v, final_norm):
    lb_all = jnp.cumsum(jax.nn.softmax(lb_logits.astype(jnp.float32), axis=0), axis=0)
    split_at = [int(v) for v in np.cumsum(IN_SPLIT_WIDTHS)[:-1]]
    cs = jax.nn.silu(c)
    for l in range(DEPTH):
        ada = cs @ w_ada[l] + b_ada[l]
        shift1, scale1, gate1, shift2, scale2, gate2 = jnp.split(ada, 6, axis=-1)
        h = modulate(rms_norm(x, norm1[l]), shift1, scale1)
        proj = h @ w_in[l]
        p_pool, p_q, p_f, p_i, p_g, g_a, g_b = jnp.split(proj, split_at, axis=-1)
        y_a = pool_mixer(p_pool, pool_w[l], pool_scale[l])
        y_b = hgrn2_mixer(p_q, p_f, p_i, p_g, lb_all[l], hg_norm[l])
        merged = jax.nn.sigmoid(g_a) * y_a + jax.nn.sigmoid(g_b) * y_b
        x = x + (1.0 + gate1)[:, None, :] * (merged @ w_out[l])
        h2 = modulate(rms_norm(x, norm2[l]), shift2, scale2)
        x = x + (1.0 + gate2)[:, None, :] * peer_ffn(h2, peer_wq[l], peer_keys[l], peer_u[l], peer_v[l])
    return rms_norm(x, final_norm)
```

```python
from contextlib import ExitStack
import numpy as np
import ml_dtypes
import concourse.bass as bass
import concourse.mybir as mybir
from concourse.bass_utils import run_bass_kernel_spmd

F32 = mybir.dt.float32
BF16 = mybir.dt.bfloat16
AF = mybir.ActivationFunctionType
ALU = mybir.AluOpType
AX = mybir.AxisListType

D = 2048
SEQ = 2048
TB = 512
NBLK = SEQ // TB
NTT = TB // 128
EPS = 1e-6
NEG = -1.0e30
SAME_ENGINE_RAW = True


class Buf:
    __slots__ = ("name", "w", "r")

    def __init__(self, name):
        self.name = name
        self.w = {}
        self.r = {}


class Prog:
    ENGS = ("sync", "scalar", "vector", "gpsimd", "tensor")

    def __init__(self, nc, stack):
        self.nc = nc
        self.stack = stack
        self.sems = {}
        self.cnt = {}
        self.known = {e: {} for e in self.ENGS}
        self.ops = []
        self.nbuf = 0
        self.base = {}
        for e in self.ENGS[1:]:
            self._sem("E_" + e)

    def _sem(self, key):
        if key not in self.sems:
            self.sems[key] = self.stack.enter_context(self.nc.semaphore(key))
            self.cnt[key] = 0
        return self.sems[key]

    def buf(self, name=None):
        self.nbuf += 1
        b = Buf(name or f"b{self.nbuf}")
        b.w = dict(self.base)
        return b

    def fence(self):
        self.base = {k: v for k, v in self.cnt.items() if v > 0}

    def bufs(self, n, name="b"):
        return [self.buf(f"{name}{i}") for i in range(n)]

    def _collect(self, eng, reads, writes):
        need = {}
        own = "E_" + eng

        def add(d, raw):
            for k, v in d.items():
                if k == own and (eng == "tensor" or not SAME_ENGINE_RAW):
                    continue
                if need.get(k, 0) < v:
                    need[k] = v

        for b in reads:
            add(b.w, True)
        for b in writes:
            add(b.w, False)
            add(b.r, False)
        kn = self.known[eng]
        waits = []
        for k, v in need.items():
            if kn.get(k, 0) >= v:
                continue
            kn[k] = v
            waits.append((k, v))
        return waits

    def op(self, eng, fn, reads=(), writes=()):
        waits = self._collect(eng, reads, writes)
        key = "E_" + eng
        self.cnt[key] += 1
        v = self.cnt[key]
        for b in reads:
            if b.r.get(key, 0) < v:
                b.r[key] = v
        for b in writes:
            if b.w.get(key, 0) < v:
                b.w[key] = v
        self.ops.append((eng, fn, waits, (key, 1)))

    def dma(self, fn, semkey, reads=(), writes=(), eng="sync"):
        self._sem(semkey)
        waits = self._collect(eng, reads, writes)
        self.cnt[semkey] += 16
        v = self.cnt[semkey]
        for b in reads:
            if b.r.get(semkey, 0) < v:
                b.r[semkey] = v
        for b in writes:
            if b.w.get(semkey, 0) < v:
                b.w[semkey] = v
        self.ops.append((eng, fn, waits, (semkey, 16)))

    def wait_all(self, eng, bufs, also_reads=False):
        need = []
        if also_reads:
            waits = self._collect(eng, (), bufs)
        else:
            waits = self._collect(eng, bufs, ())
        self.ops.append((eng, None, waits, None))

    def emit(self, name=None):
        nc = self.nc
        ops = self.ops
        sems = self.sems
        import bisect
        waited = {}
        for (_, _, waits, _) in ops:
            for (k, v) in waits:
                if k.startswith("E_"):
                    waited.setdefault(k, set()).add(v)
        waited = {k: sorted(vs) for k, vs in waited.items()}
        base = getattr(self, "_rank_base", {})

        def rank(k, v):
            return base.get(k, 0) + bisect.bisect_right(waited.get(k, []), v)

        seqs = getattr(self, "_seq_base", {})
        plan = []
        cur = dict(seqs)
        for (eng, fn, waits, inc) in ops:
            w2 = []
            for (k, v) in waits:
                if k.startswith("E_"):
                    w2.append((k, rank(k, v)))
                else:
                    w2.append((k, v))
            sig = None
            if inc is not None:
                k, amt = inc
                if k.startswith("E_"):
                    cur[k] = cur.get(k, 0) + 1
                    n = cur[k]
                    lst = waited.get(k, [])
                    i = bisect.bisect_left(lst, n)
                    if i < len(lst) and lst[i] == n:
                        sig = (k, 1)
                else:
                    sig = (k, amt)
            plan.append((eng, fn, w2, sig))
        with nc.Block(name) as blk:
            for e in self.ENGS:
                mine = [o for o in plan if o[0] == e]
                if not mine:
                    continue

                def body(eng, mine=mine):
                    for (_, fn, waits, sig) in mine:
                        for (k, v) in waits:
                            eng.wait_ge(sems[k], v)
                        if fn is None:
                            continue
                        ins = fn(eng)
                        if sig is not None:
                            ins.then_inc(sems[sig[0]], sig[1])

                getattr(blk, e)(body)
        nsig = sum(1 for o in plan if o[3] is not None)
        self.stats = (len(plan), nsig)
        self.ops = []


def mm_group(P, out_ap, pairs, reads, writes):
    pairs = list(pairs)

    def fn(e):
        n = len(pairs)
        ins = None
        for i, (l, r) in enumerate(pairs):
            ins = e.matmul(out_ap, lhsT=l, rhs=r, start=(i == 0), stop=(i == n - 1))
        return ins

    P.op("tensor", fn, reads, writes)


def build_program(debug=None):
    nc = bass.Bass("TRN2", target_bir_lowering=False)
    dt_in = lambda n, s: nc.dram_tensor(n, s, F32, kind="ExternalInput").ap()
    x_d = dt_in("x", [SEQ, D])
    cT_d = dt_in("cT", [128, 16])
    wada_d = dt_in("wada", [24, 128, 16, 512])
    bada_d = dt_in("bada", [128, 96])
    pvec_d = dt_in("pvec", [128, 6, 16])
    fnorm_d = dt_in("fnorm", [D])
    win_d = dt_in("win", [104, 128, 16, 128])
    poolw_d = dt_in("poolw", [128, 8, 512])
    wout_d = dt_in("wout", [16, 128, 16, 128])
    wq_d = dt_in("wq", [16, 128, 16, 128])
    keysT_d = dt_in("keysT", [128, 16, 128])
    UT_d = dt_in("UT", [128, 128, 16, 128])
    V_d = dt_in("V", [128, 128, D])
    ident_d = dt_in("ident", [128, 128])
    utm_d = dt_in("utm", [128, 128])
    pinv_d = dt_in("pinv", [128, 4, 16])
    out_d = nc.dram_tensor("out", [SEQ, D], F32, kind="ExternalOutput").ap()
    x1_kind = "ExternalOutput" if debug in ("p1", "zero") else "Internal"
    x1s_d = nc.dram_tensor("x1s", [SEQ, D], F32, kind=x1_kind).ap()
    g1s_d = nc.dram_tensor("g1s", [D], F32, kind=x1_kind).ap()
    g2s_d = nc.dram_tensor("g2s", [D], F32, kind=x1_kind).ap()
    dbg_d = None
    if debug == "p0":
        dbg_d = nc.dram_tensor("dbg", [128, 160], F32, kind="ExternalOutput").ap()

    with ExitStack() as gs:
        P = Prog(nc, gs)

        uid = [0]

        def mk_alloc(stack):
            def sb(name, shape, dt=F32):
                uid[0] += 1
                return stack.enter_context(nc.sbuf_tensor(f"{name}_{uid[0]}", shape, dt))
            return sb

        gsb = mk_alloc(gs)
        identf = gsb("identf", [128, 128])
        identb = gsb("identb", [128, 128], BF16)
        utm = gsb("utm_sb", [128, 128])
        onesf = gsb("onesf", [128, 128])
        onesb = gsb("onesb", [128, 128], BF16)
        pinv = gsb("pinv_sb", [128, 4, 16])
        pv = gsb("pv", [128, 6, 16])
        par = gsb("par", [128, 12, 16])
        A1, SH1, G1C, A2, SH2, G2C, PSC, HGN, LB, OML = range(10)
        epsc = gsb("epsc", [128, 1])
        ps = [gs.enter_context(nc.psum_tensor(f"ps{i}", [128, 512], F32)) for i in range(4)]
        psy = gs.enter_context(nc.psum_tensor("psy", [128, 2048], F32))
        psB = [P.buf(f"ps{i}") for i in range(4)]
        psyB = [P.buf(f"psy{i}") for i in range(4)]

        def psyb(i):
            return psy[:, i * 512:(i + 1) * 512]

        B_const = P.buf("consts")
        B_par = P.buf("par")

        with ExitStack() as s0:
            sb = mk_alloc(s0)
            NWA = 4
            wa = [sb(f"wa{i}", [128, 16, 512], BF16) for i in range(NWA)]
            waB = P.bufs(NWA, "wa")
            csb = sb("csb", [128, 16], BF16)
            cst = sb("cst", [128, 16])
            cs = sb("cs", [128, 16])
            bada = sb("bada_sb", [128, 96])
            ada = sb("ada_sb", [128, 96])
            tmpc = sb("tmpc", [128, 16])
            B_c = P.buf("c")
            B_ada = P.buf("ada")
            for (dst, src) in ((identf, ident_d), (utm, utm_d), (pinv, pinv_d), (pv, pvec_d),
                               (cst, cT_d), (bada, bada_d)):
                P.dma((lambda d, s: (lambda e: e.dma_start(out=d[:], in_=s)))(dst, src), "d_const",
                      writes=[B_const])
            P.op("vector", lambda e: e.tensor_copy(out=identb[:], in_=identf[:]), [B_const], [B_const])
            P.op("gpsimd", lambda e: e.memset(onesf[:], 1.0), [], [B_const])
            P.op("gpsimd", lambda e: e.memset(onesb[:], 1.0), [], [B_const])
            P.op("gpsimd", lambda e: e.memset(epsc[:], EPS), [], [B_const])
            P.op("scalar", lambda e: e.activation(out=cs[:], in_=cst[:], func=AF.Silu), [B_const], [B_c])
            P.op("vector", lambda e: e.tensor_copy(out=csb[:], in_=cs[:]), [B_c], [B_c])
            for j in range(24):
                P.dma((lambda j: (lambda e: e.dma_start(out=wa[j % NWA][:], in_=wada_d[j])))(j), f"d_wa{j % NWA}",
                      writes=[waB[j % NWA]], eng="gpsimd")

                def fn(e, j=j):
                    ins = None
                    for q in range(4):
                        col = j * 4 + q
                        for kc in range(16):
                            ins = e.matmul(ps[0][:, col:col + 1], lhsT=wa[j % NWA][:, kc, q * 128:(q + 1) * 128],
                                           rhs=csb[:, kc:kc + 1], start=(kc == 0), stop=(kc == 15))
                    return ins
                P.op("tensor", fn, [waB[j % NWA], B_c], [psB[0]])
            P.op("vector", lambda e: e.tensor_tensor(out=ada[:], in0=ps[0][:, 0:96], in1=bada[:], op=ALU.add),
                 [psB[0], B_const], [B_ada])
            P.op("vector", lambda e: e.scalar_tensor_tensor(out=par[:, A1, :], in0=ada[:, 16:32], scalar=1.0,
                                                            in1=pv[:, 0, :], op0=ALU.add, op1=ALU.mult),
                 [B_ada, B_const], [B_par])
            P.op("vector", lambda e: e.tensor_copy(out=par[:, SH1, :], in_=ada[:, 0:16]), [B_ada], [B_par])
            P.op("vector", lambda e: e.tensor_scalar(out=par[:, G1C, :], in0=ada[:, 32:48], scalar1=1.0,
                                                     scalar2=None, op0=ALU.add), [B_ada], [B_par])
            P.op("vector", lambda e: e.scalar_tensor_tensor(out=par[:, A2, :], in0=ada[:, 64:80], scalar=1.0,
                                                            in1=pv[:, 1, :], op0=ALU.add, op1=ALU.mult),
                 [B_ada, B_const], [B_par])
            P.op("vector", lambda e: e.tensor_copy(out=par[:, SH2, :], in_=ada[:, 48:64]), [B_ada], [B_par])
            P.op("vector", lambda e: e.tensor_scalar(out=par[:, G2C, :], in0=ada[:, 80:96], scalar1=1.0,
                                                     scalar2=None, op0=ALU.add), [B_ada], [B_par])
            P.op("vector", lambda e: e.tensor_copy(out=par[:, PSC, :], in_=pv[:, 2, :]), [B_const], [B_par])
            P.op("vector", lambda e: e.tensor_copy(out=par[:, HGN, :], in_=pv[:, 3, :]), [B_const], [B_par])
            B_tc = P.buf("tmpc")
            P.op("vector", lambda e: e.tensor_tensor(out=tmpc[:], in0=pv[:, 4, :], in1=pv[:, 5, :], op=ALU.subtract),
                 [B_const], [B_tc])
            P.op("scalar", lambda e: e.activation(out=par[:, LB, :], in_=tmpc[:], func=AF.Sigmoid), [B_tc], [B_par])
            P.op("vector", lambda e: e.tensor_scalar(out=par[:, OML, :], in0=par[:, LB, :], scalar1=-1.0, scalar2=1.0,
                                                     op0=ALU.mult, op1=ALU.add), [B_par], [B_par])
            B_gs = P.buf("gs")
            P.dma(lambda e: e.dma_start(out=g1s_d.rearrange("(j p) -> p j", p=128), in_=par[:, G1C, :],
                                        allow_slow_non_contiguous=True), "d_gs", reads=[B_par], writes=[B_gs])
            P.dma(lambda e: e.dma_start(out=g2s_d.rearrange("(j p) -> p j", p=128), in_=par[:, G2C, :],
                                        allow_slow_non_contiguous=True), "d_gs", reads=[B_par], writes=[B_gs])
            outs = [B_gs]
            if debug == "p0":
                B_dbg = P.buf("dbg")
                P.dma(lambda e: e.dma_start(out=dbg_d[:, 0:160], in_=par[:, 0:10, :].rearrange("p a b -> p (a b)")),
                      "d_dbg", reads=[B_par], writes=[B_dbg])
                outs.append(B_dbg)
            P.wait_all("sync", outs)
            P.fence()
        if debug == "p0":
            P.emit("main")
            return nc

        with ExitStack() as s1:
            sb = mk_alloc(s1)
            xt = sb("xt", [128, NTT, D])
            hT = sb("hT", [128, 16, TB], BF16)
            mT = sb("mT", [128, 16, TB], BF16)
            pe = sb("pe", [128, 8, 16 + TB])
            pooledT = sb("pooledT", [128, 8, TB], BF16)
            Sst = sb("Sst", [128, 16, 128])
            g1bc = sb("g1bc", [128, D])
            NW = 6
            PF = 4
            wbf = [sb(f"wbf{i}", [128, 16, 128], BF16) for i in range(NW)]
            poolw_b = sb("poolw_b", [128, 8, 512], BF16)
            psT = ps[3][:].bitcast(BF16)
            W = {}
            WB = {}
            alias = {"sig": 0, "f": 0, "lf": 1, "kk": 2, "cum": 3, "cumref": 4, "eq": 5, "ek": 6,
                     "sd": 7, "rstd": 7, "t1": 8, "t2": 8, "t3": 8, "u1": 9}
            Tt = [sb(f"wT{i}", [128, TB]) for i in range(10)]
            TBf = [P.buf(f"wT{i}") for i in range(10)]
            G3 = [[sb(f"g3_{i}_{j}", [128, TB], BF16) for j in range(3)] for i in range(2)]
            B_G3 = [[P.buf(f"g3_{i}_{j}") for j in range(3)] for i in range(2)]
            qr = sb("qr", [128, TB])
            B_qr = P.buf("qr")
            for n, i in alias.items():
                W[n] = Tt[i]
                WB[n] = TBf[i]
            qtb = sb("qtb", [128, TB], BF16)
            ktb = sb("ktb", [128, TB], BF16)
            iTb = sb("iTb", [128, TB], BF16)
            osqb = sb("osqb", [128, TB], BF16)
            tok = sb("tok", [128, 8, 128], BF16)
            attm = [sb(f"attm{i}", [128, 128], BF16) for i in range(4)]
            Sp = [sb(f"Sp{i}", [128, 128], BF16) for i in range(2)]
            tmpKI = [sb(f"tmpKI{i}", [128, 128]) for i in range(4)]
            esc = sb("esc", [128, 12])
            xnb = [sb(f"xn{i}", [128, D]) for i in range(2)]
            xn = xnb[0]
            statb = [sb(f"stat{i}", [128, 4]) for i in range(2)]
            pA = [sb(f"pA{i}", [128, 16 + TB]) for i in range(2)]
            o2t = sb("o2t", [128, NTT, 128])
            B_qtb, B_ktb, B_iTb, B_osqb, B_tok, B_esc, B_xn, B_stat, B_o2t = (
                P.buf(n) for n in ("qtb", "ktb", "iTb", "osqb", "tok", "esc", "xn", "stat", "o2t"))
            B_attm = P.bufs(4, "attm")
            B_Sp = P.bufs(2, "Sp")
            B_tmpKI = P.bufs(4, "tmpKI")
            B_pA = P.bufs(2, "pA")
            B_xt = P.bufs(NTT, "xt")
            B_hT = P.buf("hT")
            B_mT = P.buf("mT")
            B_pe = P.bufs(8, "pe")
            B_pooledT = P.buf("pooledT")
            B_S = P.bufs(16, "S")
            B_g1bc = P.buf("g1bc")
            B_wbf = P.bufs(NW, "wbf")
            B_poolw = P.buf("poolw")
            B_x1s = P.buf("x1s")
            wctr = [0]
            pctr = [0]

            blk_srcs = ([("win", c) for c in range(8)]
                        + [("win", base + hd) for hd in range(16) for base in (24, 88, 72, 56, 8, 40)]
                        + [("wout", n) for n in range(16)])
            all_srcs = blk_srcs * NBLK
            wiss = [0]

            def wstream(kind, idx):
                i = wctr[0]
                assert all_srcs[i] == (kind, idx), (i, all_srcs[i], kind, idx)
                while wiss[0] < min(len(all_srcs), i + 1 + PF):
                    j = wiss[0]
                    wiss[0] += 1
                    sj = j % NW
                    kd, ix = all_srcs[j]
                    src = win_d[ix] if kd == "win" else wout_d[ix]
                    P.dma((lambda sj, src: (lambda e: e.dma_start(out=wbf[sj][:], in_=src)))(sj, src), f"d_wbf{sj}",
                          writes=[B_wbf[sj]], eng="gpsimd")
                wctr[0] += 1
                s = i % NW
                return wbf[s], B_wbf[s]

            def proj(kind, idx, act_T, B_act):
                wb, wB = wstream(kind, idx)
                k = pctr[0] % 3
                pctr[0] += 1
                mm_group(P, ps[k][:, :], [(wb[:, kc, :], act_T[:, kc, :]) for kc in range(16)],
                         [wB, B_act], [psB[k]])
                return ps[k], psB[k]

            B_xnb = [B_xn, P.buf("xn1")]
            B_statb = [B_stat, P.buf("stat1")]

            def rms_to_T(tt, src, B_src, a_idx, sh_idx, dstT, B_dst):
                xn, B_xn, stat, B_stat = xnb[tt % 2], B_xnb[tt % 2], statb[tt % 2], B_statb[tt % 2]
                P.op("scalar", lambda e: e.activation(out=xn[:], in_=src, func=AF.Square), [B_src], [B_xn])
                P.op("vector", lambda e: e.tensor_reduce(out=stat[:, 0:1], in_=xn[:], axis=AX.X, op=ALU.add),
                     [B_xn], [B_stat])
                P.op("scalar", lambda e: e.activation(out=stat[:, 1:2], in_=stat[:, 0:1], func=AF.Ln,
                                                      scale=1.0 / D, bias=epsc[:, 0:1]), [B_stat, B_const], [B_stat])
                P.op("scalar", lambda e: e.activation(out=stat[:, 2:3], in_=stat[:, 1:2], func=AF.Exp, scale=-0.5),
                     [B_stat], [B_stat])
                P.op("vector", lambda e: e.tensor_scalar(out=xn[:], in0=src, scalar1=stat[:, 2:3], scalar2=None,
                                                         op0=ALU.mult), [B_src, B_stat], [B_xn])
                for gq in range(4):
                    k = pctr[0] % 3
                    pctr[0] += 1

                    def tfn(e, gq=gq, k=k):
                        ins = None
                        for j in range(4):
                            dc = gq * 4 + j
                            ins = e.transpose(out=ps[k][:, j * 128:(j + 1) * 128], in_=xn[:, dc * 128:(dc + 1) * 128],
                                              identity=identf[:])
                        return ins
                    P.op("tensor", tfn, [B_xn, B_const], [psB[k]])
                    for j in range(4):
                        dc = gq * 4 + j
                        o_ap = dstT[:, dc, tt * 128:(tt + 1) * 128]
                        i_ap = ps[k][:, j * 128:(j + 1) * 128]
                        if j % 2 == 0:
                            P.op("scalar", (lambda o_ap, i_ap, dc: (lambda e: e.activation(
                                out=o_ap, in_=i_ap, func=AF.Identity, scale=par[:, a_idx, dc:dc + 1],
                                bias=par[:, sh_idx, dc:dc + 1])))(o_ap, i_ap, dc), [psB[k], B_par], [B_dst])
                        else:
                            P.op("vector", (lambda o_ap, i_ap, dc: (lambda e: e.tensor_scalar(
                                out=o_ap, in0=i_ap, scalar1=par[:, a_idx, dc:dc + 1], scalar2=par[:, sh_idx, dc:dc + 1],
                                op0=ALU.mult, op1=ALU.add)))(o_ap, i_ap, dc), [psB[k], B_par], [B_dst])

            with ExitStack() as s1p:
                sbp = mk_alloc(s1p)
                xn4 = xn[:].rearrange("p (a b) -> p a b", a=4)
                for hf in range(2):
                    P.dma((lambda hf: (lambda e: e.dma_start(out=xn4, in_=poolw_d[:, hf * 4:hf * 4 + 4, :])))(hf), "d_pw",
                          writes=[B_xn])
                    P.op("vector", (lambda hf: (lambda e: e.tensor_copy(out=poolw_b[:, hf * 4:hf * 4 + 4, :], in_=xn4)))(hf),
                         [B_xn], [B_poolw])
                P.dma(lambda e: e.dma_start(out=g1bc[:], in_=g1s_d.partition_broadcast(128)), "d_g1bc",
                      writes=[B_g1bc])
                P.op("gpsimd", lambda e: e.memset(Sst[:], 0.0), [], B_S)
                P.op("gpsimd", lambda e: e.memset(pe[:], 0.0), [], B_pe)
                for i in range(2):
                    P.op("gpsimd", (lambda i: (lambda e: e.memset(pA[i][:], 0.0)))(i), [], [B_pA[i]])
                for i in range(4):
                    P.op("gpsimd", (lambda i: (lambda e: e.memset(attm[i][:], 0.0)))(i), [], [B_attm[i]])
                P.fence()

            for blk in range(NBLK):
                t0 = blk * TB
                for tt in range(NTT):
                    P.dma((lambda tt, t0: (lambda e: e.dma_start(out=xt[:, tt, :],
                                                                 in_=x_d[t0 + tt * 128:t0 + (tt + 1) * 128, :])))(tt, t0),
                          f"d_xt{tt}", writes=[B_xt[tt]])
                for tt in range(NTT):
                    rms_to_T(tt, xt[:, tt, :], B_xt[tt], A1, SH1, hT, B_hT)
                for c in range(8):
                    pp, pB = proj("win", c, hT, B_hT)
                    P.op("scalar", (lambda c, pp: (lambda e: e.copy(out=pe[:, c, 16:16 + TB], in_=pp[:, :])))(c, pp),
                         [pB], [B_pe[c]])
                    g = c // 2
                    w = (2, 4, 8, 16)[g]
                    cur = pe[:, c, :]
                    curB = B_pe[c]
                    sh = 1
                    i = 0
                    while sh < w:
                        dst = pA[i % 2]
                        dB = B_pA[i % 2]
                        P.op("gpsimd", (lambda dst, cur, sh: (lambda e: e.tensor_tensor(
                            out=dst[:, sh:16 + TB], in0=cur[:, sh:16 + TB], in1=cur[:, 0:16 + TB - sh], op=ALU.add)))(
                            dst, cur, sh), [curB], [dB])
                        cur = dst
                        curB = dB
                        sh *= 2
                        i += 1
                    P.op("vector", (lambda c, cur, w: (lambda e: e.scalar_tensor_tensor(
                        out=pooledT[:, c, :], in0=cur[:, 16:16 + TB], scalar=1.0 / w, in1=pe[:, c, 16:16 + TB],
                        op0=ALU.mult, op1=ALU.subtract)))(c, cur, w), [curB, B_pe[c]], [B_pooledT])
                    if blk == 0:
                        P.op("vector", (lambda c, cur, g: (lambda e: e.tensor_tensor(
                            out=cur[:, 16:32], in0=cur[:, 16:32], in1=pinv[:, g, :], op=ALU.mult)))(c, cur, g),
                            [curB, B_const], [curB])
                        P.op("vector", (lambda c, cur: (lambda e: e.tensor_tensor(
                            out=pooledT[:, c, 0:16], in0=cur[:, 16:32], in1=pe[:, c, 16:32], op=ALU.subtract)))(c, cur),
                            [curB, B_pe[c]], [B_pooledT])
                    P.op("gpsimd", (lambda c: (lambda e: e.tensor_copy(out=pe[:, c, 0:16], in_=pe[:, c, TB:TB + 16])))(c),
                         [B_pe[c]], [B_pe[c]])
                for hd in range(16):
                    par2 = hd % 2
                    sg, sbg, sa = G3[par2]
                    B_sg, B_sbg, B_sa = B_G3[par2]
                    def early(hd1, j):
                        tgt, B_tgt = ((W["sig"], WB["sig"]), (G3[hd1 % 2][1], B_G3[hd1 % 2][1]),
                                      (G3[hd1 % 2][2], B_G3[hd1 % 2][2]))[j]
                        pp, pB = proj("win", (24, 88, 72)[j] + hd1, hT, B_hT)
                        P.op("scalar", (lambda pp, tgt: (lambda e: e.activation(out=tgt[:], in_=pp[:, :],
                                                                                func=AF.Sigmoid)))(pp, tgt), [pB], [B_tgt])
                    if hd == 0:
                        for j in range(3):
                            early(0, j)
                    gp, gB = proj("win", 56 + hd, hT, B_hT)
                    P.op("scalar", (lambda gp, sg: (lambda e: e.activation(out=sg[:], in_=gp[:, :], func=AF.Silu)))(gp, sg),
                         [gB], [B_sg])
                    P.op("vector", (lambda hd: (lambda e: e.tensor_scalar(
                        out=W["f"][:], in0=W["sig"][:], scalar1=par[:, OML, hd:hd + 1], scalar2=par[:, LB, hd:hd + 1],
                        op0=ALU.mult, op1=ALU.add)))(hd), [WB["sig"], B_par], [WB["f"]])
                    P.op("scalar", lambda e: e.activation(out=W["lf"][:], in_=W["f"][:], func=AF.Ln),
                         [WB["f"]], [WB["lf"]])
                    P.op("gpsimd", lambda e: e.tensor_scalar(out=W["kk"][:], in0=W["f"][:], scalar1=-1.0, scalar2=1.0,
                                                             op0=ALU.mult, op1=ALU.add), [WB["f"]], [WB["kk"]])
                    for c in range(NTT):
                        P.op("vector", (lambda c: (lambda e: e.tensor_tensor_scan(
                            out=W["cum"][:, c * 128:(c + 1) * 128], data0=onesf[:], data1=W["lf"][:, c * 128:(c + 1) * 128],
                            initial=0.0, op0=ALU.mult, op1=ALU.add)))(c), [WB["lf"], B_const], [WB["cum"]])
                    qp, qB = proj("win", 8 + hd, hT, B_hT)
                    P.op("scalar", (lambda qp: (lambda e: e.copy(out=qr[:], in_=qp[:, :])))(qp), [qB], [B_qr])
                    ip, iB = proj("win", 40 + hd, hT, B_hT)
                    P.op("scalar", (lambda ip: (lambda e: e.copy(out=iTb[:], in_=ip[:, :])))(ip), [iB], [B_iTb])
                    cum3 = W["cum"][:].rearrange("p (c t) -> p c t", c=NTT)
                    cr3 = W["cumref"][:].rearrange("p (c t) -> p c t", c=NTT)
                    P.op("gpsimd", lambda e: e.tensor_tensor(out=cr3, in0=cum3,
                                                             in1=cum3[:, :, 63:64].to_broadcast([128, NTT, 128]),
                                                             op=ALU.subtract), [WB["cum"]], [WB["cumref"]])
                    P.op("scalar", lambda e: e.activation(out=W["eq"][:], in_=W["cumref"][:], func=AF.Exp),
                         [WB["cumref"]], [WB["eq"]])
                    P.op("scalar", lambda e: e.activation(out=W["ek"][:], in_=W["cumref"][:], func=AF.Exp, scale=-1.0),
                         [WB["cumref"]], [WB["ek"]])
                    P.op("scalar", lambda e: e.activation(out=esc[:, 0:4], in_=cum3[:, :, 63], func=AF.Exp),
                         [WB["cum"]], [B_esc])
                    P.op("scalar", lambda e: e.activation(out=esc[:, 4:8], in_=cum3[:, :, 127], func=AF.Exp),
                         [WB["cum"]], [B_esc])
                    P.op("scalar", lambda e: e.activation(out=esc[:, 8:12], in_=cr3[:, :, 127], func=AF.Exp),
                         [WB["cumref"]], [B_esc])
                    P.op("gpsimd", lambda e: e.tensor_tensor(out=ktb[:], in0=W["kk"][:], in1=W["ek"][:], op=ALU.mult),
                         [WB["kk"], WB["ek"]], [B_ktb])
                    P.op("vector", lambda e: e.tensor_tensor(out=qtb[:], in0=qr[:], in1=W["eq"][:], op=ALU.mult),
                         [B_qr, WB["eq"]], [B_qtb])

                    def tfn(e):
                        ins = None
                        for c in range(NTT):
                            ins = e.transpose(out=psT[:, c * 128:(c + 1) * 128], in_=iTb[:, c * 128:(c + 1) * 128],
                                              identity=identb[:])
                        for c in range(NTT):
                            ins = e.transpose(out=psT[:, (4 + c) * 128:(5 + c) * 128], in_=ktb[:, c * 128:(c + 1) * 128],
                                              identity=identb[:])
                        return ins
                    P.op("tensor", tfn, [B_iTb, B_ktb, B_const], [psB[3]])
                    P.op("vector", lambda e: e.tensor_copy(out=tok[:].rearrange("p a b -> p (a b)"), in_=psT[:, :]),
                         [psB[3]], [B_tok])
                    g = hd // 4
                    e0 = (hd % 4) * 128
                    kya = pctr[0] % 3
                    pctr[0] += 1
                    mm_group(P, ps[kya][:, :], [(poolw_b[:, g * 2 + kc, e0:e0 + 128], pooledT[:, g * 2 + kc, :]) for kc in range(2)],
                             [B_poolw, B_pooledT], [psB[kya]])
                    P.op("vector", (lambda hd, kya, sa: (lambda e: e.scalar_tensor_tensor(
                        out=W["u1"][:], in0=ps[kya][:, :], scalar=par[:, PSC, hd:hd + 1], in1=sa[:],
                        op0=ALU.mult, op1=ALU.mult)))(hd, kya, sa), [psB[kya], B_sa, B_par], [WB["u1"]])
                    for c in range(NTT):
                        def afn(e, c=c):
                            b0 = c * 128
                            e.matmul(psyb(0)[:, b0 + 64:b0 + 128], lhsT=ktb[:, b0:b0 + 128], rhs=qtb[:, b0 + 64:b0 + 128],
                                     start=True, stop=True)
                            return e.matmul(psyb(0)[0:64, b0:b0 + 64], lhsT=ktb[:, b0:b0 + 64], rhs=qtb[:, b0:b0 + 64],
                                            start=True, stop=True)
                        P.op("tensor", afn, [B_ktb, B_qtb], [psyB[0]])
                        P.op("vector", (lambda c: (lambda e: e.tensor_tensor(
                            out=attm[c][:, 64:128], in0=psyb(0)[:, c * 128 + 64:c * 128 + 128], in1=utm[:, 64:128],
                            op=ALU.mult)))(c), [psyB[0], B_const], [B_attm[c]])
                        P.op("vector", (lambda c: (lambda e: e.tensor_tensor(
                            out=attm[c][0:64, 0:64], in0=psyb(0)[0:64, c * 128:c * 128 + 64], in1=utm[0:64, 0:64],
                            op=ALU.mult)))(c), [psyB[0], B_const], [B_attm[c]])
                    for c in range(NTT):
                        cs_ = slice(c * 128, (c + 1) * 128)
                        P.op("tensor", (lambda c, cs_: (lambda e: e.matmul(psyb(2)[:, cs_], lhsT=tok[:, 4 + c, :],
                                                                           rhs=tok[:, c, :], start=True, stop=True)))(c, cs_),
                             [B_tok], [psyB[2]])
                        P.op("vector", (lambda c, cs_: (lambda e: e.tensor_scalar(
                            out=tmpKI[c][:], in0=psyb(2)[:, cs_], scalar1=esc[:, 8 + c:9 + c], scalar2=None,
                            op0=ALU.mult)))(c, cs_), [psyB[2], B_esc], [B_tmpKI[c]])
                    for c in range(NTT):
                        cs_ = slice(c * 128, (c + 1) * 128)
                        P.op("vector", (lambda c, hd: (lambda e: e.tensor_scalar(
                            out=Sp[c % 2][:], in0=Sst[:, hd, :], scalar1=esc[:, c:c + 1], scalar2=None, op0=ALU.mult)))(c, hd),
                            [B_S[hd], B_esc], [B_Sp[c % 2]])

                        def ofn(e, c=c, cs_=cs_):
                            e.matmul(psyb(1)[:, cs_], lhsT=tok[:, c, :], rhs=attm[c][:], start=True, stop=False)
                            return e.matmul(psyb(1)[:, cs_], lhsT=Sp[c % 2][:], rhs=qtb[:, cs_], start=False, stop=True)
                        P.op("tensor", ofn, [B_tok, B_attm[c], B_Sp[c % 2], B_qtb], [psyB[1]])
                        if c < 3 and hd + 1 < 16:
                            early(hd + 1, c)
                        if not (blk == NBLK - 1 and c == NTT - 1):
                            P.op("vector", (lambda c, hd: (lambda e: e.scalar_tensor_tensor(
                                out=Sst[:, hd, :], in0=Sst[:, hd, :], scalar=esc[:, 4 + c:5 + c], in1=tmpKI[c][:],
                                op0=ALU.mult, op1=ALU.add)))(c, hd), [B_S[hd], B_esc, B_tmpKI[c]], [B_S[hd]])
                    P.op("scalar", lambda e: e.activation(out=osqb[:], in_=psyb(1), func=AF.Square), [psyB[1]], [B_osqb])
                    P.op("tensor", lambda e: e.matmul(psyb(3), lhsT=onesb[:], rhs=osqb[:], start=True, stop=True),
                         [B_osqb, B_const], [psyB[3]])
                    P.op("scalar", lambda e: e.activation(out=W["sd"][:], in_=psyb(3), func=AF.Ln, scale=1.0 / 128,
                                                          bias=epsc[:, 0:1]), [psyB[3], B_const], [WB["sd"]])
                    P.op("scalar", lambda e: e.activation(out=W["rstd"][:], in_=W["sd"][:], func=AF.Exp, scale=-0.5),
                         [WB["sd"]], [WB["rstd"]])
                    P.op("vector", lambda e: e.tensor_tensor(out=W["t1"][:], in0=psyb(1), in1=W["rstd"][:], op=ALU.mult),
                         [psyB[1], WB["rstd"]], [WB["t1"]])
                    P.op("vector", (lambda hd, sg: (lambda e: e.scalar_tensor_tensor(
                        out=W["t2"][:], in0=W["t1"][:], scalar=par[:, HGN, hd:hd + 1], in1=sg[:],
                        op0=ALU.mult, op1=ALU.mult)))(hd, sg), [WB["t1"], B_sg, B_par], [WB["t2"]])
                    P.op("gpsimd", (lambda sbg: (lambda e: e.tensor_tensor(out=W["t3"][:], in0=W["t2"][:], in1=sbg[:],
                                                                           op=ALU.mult)))(sbg),
                         [WB["t2"], B_sbg], [WB["t3"]])
                    P.op("gpsimd", (lambda hd: (lambda e: e.tensor_tensor(out=mT[:, hd, :], in0=W["u1"][:], in1=W["t3"][:],
                                                                          op=ALU.add)))(hd), [WB["u1"], WB["t3"]], [B_mT])
                for nch in range(16):
                    wb, wB = wstream("wout", nch)
                    k = pctr[0] % 3
                    pctr[0] += 1

                    def wfn(e, wb=wb, k=k):
                        ins = None
                        for tt in range(NTT):
                            for fc in range(16):
                                ins = e.matmul(ps[k][:, tt * 128:(tt + 1) * 128], lhsT=mT[:, fc, tt * 128:(tt + 1) * 128],
                                               rhs=wb[:, fc, :], start=(fc == 0), stop=(fc == 15))
                        return ins
                    P.op("tensor", wfn, [wB, B_mT], [psB[k]])
                    P.op("vector", (lambda k, nch: (lambda e: e.tensor_tensor(
                        out=o2t[:], in0=ps[k][:, :].rearrange("p (a b) -> p a b", a=NTT),
                        in1=g1bc[:, nch * 128:(nch + 1) * 128].unsqueeze(1).to_broadcast([128, NTT, 128]),
                        op=ALU.mult)))(k, nch), [psB[k], B_g1bc], [B_o2t])
                    P.op("gpsimd", (lambda nch: (lambda e: e.tensor_tensor(
                        out=xt[:, :, nch * 128:(nch + 1) * 128], in0=xt[:, :, nch * 128:(nch + 1) * 128], in1=o2t[:],
                        op=ALU.add)))(nch), [B_o2t] + B_xt, B_xt)
                for tt in range(NTT):
                    P.dma((lambda tt, t0: (lambda e: e.dma_start(out=x1s_d[t0 + tt * 128:t0 + (tt + 1) * 128, :],
                                                                 in_=xt[:, tt, :])))(tt, t0), "d_x1s", reads=[B_xt[tt]],
                          writes=[B_x1s])
                P.wait_all("sync", [B_x1s])
                P.fence()
        if debug == "p1":
            P.emit("main")
            return nc

        THR = 1.0 - 2.0e-6
        with ExitStack() as s2:
            sb = mk_alloc(s2)
            hT = sb("h2T", [128, 16, TB], BF16)
            qM = sb("qM", [128, 16 * TB], BF16)
            qT = qM[:].rearrange("p (g t) -> p g t", g=16)
            Mb1 = sb("Mb1", [128, 16 * TB], BF16)
            Mv = [qM[:].rearrange("p (tt h a b) -> p tt h a b", tt=NTT, h=8, a=2),
                  Mb1[:].rearrange("p (tt h a b) -> p tt h a b", tt=NTT, h=8, a=2)]
            E1b = sb("E1b", [128, NTT, 8, 128], BF16)
            E0p = sb("E0p", [128, NTT, 8, 128])
            Dm = sb("Dm", [128, NTT, 8, 128], BF16)
            yacc = sb("yacc", [128, NTT, D])
            with ExitStack() as sA:
                sba = mk_alloc(sA)
                NWQ = 4
                a_wbf = [sba(f"wbf{i}", [128, 16, 128], BF16) for i in range(NWQ)]
                a_xtmp = [sba(f"xtmp{i}", [128, D]) for i in range(2)]
                a_xn = [sba(f"xn{i}", [128, D]) for i in range(2)]
                a_stat = [sba(f"stat{i}", [128, 4]) for i in range(2)]
                a_Ef = sba("Ef", [128, 16, 128])
                a_Ebt = sba("Ebt", [128, 16, 128], BF16)
                a_wk = sba("wk", [128, 16, 128])
                a_top = sba("top", [128, 16, 16])
                a_cand = sba("cand", [128, 8, 256])
                a_ctop = sba("ctop", [128, 8, 16])
                a_smax = sba("smax", [128, 16])
                a_zz = sba("zz", [128, 32])
                a_kst = sba("kst", [128, 16, 128])
                a_keysb = sba("keysb", [128, 16, 128], BF16)
            with ExitStack() as sM:
                sbm = mk_alloc(sM)
                m_utb = [sbm(f"utb{i}", [128, 16, 128], BF16) for i in range(6)]
                m_vb = [sbm(f"vb{i}", [128, D], BF16) for i in range(8)]
                m_ge = [sbm(f"ge{i}", [128, TB], BF16) for i in range(2)]
                m_AT = [sbm(f"AT{i}", [128, TB], BF16) for i in range(8)]
                m_Pf = [sbm(f"Pf{i}", [128, 8, 2, 128]) for i in range(3)]
            with ExitStack() as sE:
                sbe = mk_alloc(sE)
                e_xtmp = [sbe(f"xe{i}", [128, D]) for i in range(4)]
                e_g2bc = sbe("g2bc", [128, D])
                e_fnbc = sbe("fnbc", [128, D])
                e_junk = [sbe(f"junk{i}", [128, D]) for i in range(2)]
                e_stat = [sbe(f"state{i}", [128, 4]) for i in range(2)]

            def peer_prologue(blk):
                t0 = blk * TB
                wbf, xtmp, xn, stat, Ef, Ebt, wk, top, cand, ctop, smax, zz, kst, keysb = (
                    a_wbf, a_xtmp, a_xn, a_stat, a_Ef, a_Ebt, a_wk, a_top, a_cand, a_ctop, a_smax, a_zz, a_kst,
                    a_keysb)
                B_wbf = P.bufs(NWQ, "wbf2")
                B_xtmp = P.bufs(2, "xtmp")
                B_xn, B_stat, B_Ef, B_Ebt, B_wk, B_top, B_cand, B_ctop, B_smax, B_zz, B_kst, B_keys = (
                    P.buf(n) for n in ("xn2", "stat2", "Ef", "Ebt", "wk", "top", "cand", "ctop", "smax", "zz", "kst",
                                       "keysb"))
                B_hT = P.buf("h2T")
                B_qT = P.buf("qT")
                B_E1 = P.bufs(NTT, "E1b")
                B_E0 = P.bufs(NTT, "E0p")
                B_Dm = P.bufs(NTT, "Dm")
                P.dma(lambda e: e.dma_start(out=kst[:], in_=keysT_d), "d_kst", writes=[B_kst])
                P.op("vector", lambda e: e.tensor_copy(out=keysb[:], in_=kst[:]), [B_kst], [B_keys])
                pctr = [0]
                B_xn2 = [B_xn, P.buf("xn2b")]
                B_stat2 = [B_stat, P.buf("stat2b")]

                def norm_tt(tt, xn, stat, B_xn, B_stat):
                        P.dma((lambda tt: (lambda e: e.dma_start(out=xtmp[tt % 2][:],
                                                                 in_=x1s_d[t0 + tt * 128:t0 + (tt + 1) * 128, :])))(tt),
                              f"d_xtmp{tt % 2}", writes=[B_xtmp[tt % 2]])
                        src = xtmp[tt % 2][:]
                        B_src = B_xtmp[tt % 2]
                        P.op("scalar", (lambda src: (lambda e: e.activation(out=xn[:], in_=src, func=AF.Square)))(src),
                             [B_src], [B_xn])
                        P.op("vector", lambda e: e.tensor_reduce(out=stat[:, 0:1], in_=xn[:], axis=AX.X, op=ALU.add),
                             [B_xn], [B_stat])
                        P.op("scalar", lambda e: e.activation(out=stat[:, 1:2], in_=stat[:, 0:1], func=AF.Ln,
                                                              scale=1.0 / D, bias=epsc[:, 0:1]), [B_stat, B_const], [B_stat])
                        P.op("scalar", lambda e: e.activation(out=stat[:, 2:3], in_=stat[:, 1:2], func=AF.Exp, scale=-0.5),
                             [B_stat], [B_stat])
                        P.op("vector", (lambda src: (lambda e: e.tensor_scalar(out=xn[:], in0=src, scalar1=stat[:, 2:3],
                                                                               scalar2=None, op0=ALU.mult)))(src),
                             [B_src, B_stat], [B_xn])
                        for gq in range(4):
                            k = pctr[0] % 3
                            pctr[0] += 1

                            def tfn(e, gq=gq, k=k):
                                ins = None
                                for j in range(4):
                                    dc = gq * 4 + j
                                    ins = e.transpose(out=ps[k][:, j * 128:(j + 1) * 128],
                                                      in_=xn[:, dc * 128:(dc + 1) * 128], identity=identf[:])
                                return ins
                            P.op("tensor", tfn, [B_xn, B_const], [psB[k]])
                            for j in range(4):
                                dc = gq * 4 + j
                                o_ap = hT[:, dc, tt * 128:(tt + 1) * 128]
                                i_ap = ps[k][:, j * 128:(j + 1) * 128]
                                if j % 2 == 0:
                                    P.op("scalar", (lambda o_ap, i_ap, dc: (lambda e: e.activation(
                                        out=o_ap, in_=i_ap, func=AF.Identity, scale=par[:, A2, dc:dc + 1],
                                        bias=par[:, SH2, dc:dc + 1])))(o_ap, i_ap, dc), [psB[k], B_par], [B_hT])
                                else:
                                    P.op("vector", (lambda o_ap, i_ap, dc: (lambda e: e.tensor_scalar(
                                        out=o_ap, in0=i_ap, scalar1=par[:, A2, dc:dc + 1], scalar2=par[:, SH2, dc:dc + 1],
                                        op0=ALU.mult, op1=ALU.add)))(o_ap, i_ap, dc), [psB[k], B_par], [B_hT])

                for tt in range(NTT):
                    norm_tt(tt, xn[tt % 2], stat[tt % 2], B_xn2[tt % 2], B_stat2[tt % 2])
                for ch in range(16):
                    s = ch % NWQ
                    if ch == 0:
                        for c2 in range(min(3, 16)):
                            P.dma((lambda c2: (lambda e: e.dma_start(out=wbf[c2 % NWQ][:], in_=wq_d[c2])))(c2),
                                  f"d_wbfq{c2 % NWQ}", writes=[B_wbf[c2 % NWQ]], eng="gpsimd")
                    if ch + 3 < 16:
                        c2 = ch + 3
                        P.dma((lambda c2: (lambda e: e.dma_start(out=wbf[c2 % NWQ][:], in_=wq_d[c2])))(c2),
                              f"d_wbfq{c2 % NWQ}", writes=[B_wbf[c2 % NWQ]], eng="gpsimd")
                    k = pctr[0] % 3
                    pctr[0] += 1
                    mm_group(P, ps[k][:, :], [(wbf[s][:, kc, :], hT[:, kc, :]) for kc in range(16)],
                             [B_wbf[s], B_hT], [psB[k]])
                    P.op("scalar", (lambda ch, k: (lambda e: e.copy(out=qT[:, ch, :], in_=ps[k][:, :])))(ch, k),
                         [psB[k]], [B_qT])
                for tt in range(NTT):
                    for bk in range(4):
                        def sfn(e, tt=tt, bk=bk):
                            ins = None
                            for j in range(4):
                                g = bk * 4 + j
                                ins = e.matmul(psyb(bk)[:, j * 128:(j + 1) * 128], lhsT=qT[:, g, tt * 128:(tt + 1) * 128],
                                               rhs=keysb[:, g, :], start=True, stop=True)
                            return ins
                        P.op("tensor", sfn, [B_qT, B_keys], [psyB[bk]])
                        pv3 = psyb(bk).rearrange("p (a b) -> p a b", a=4)
                        P.op("vector", (lambda bk, pv3: (lambda e: e.tensor_reduce(
                            out=smax[:, bk * 4:bk * 4 + 4], in_=pv3, axis=AX.X, op=ALU.max)))(bk, pv3),
                            [psyB[bk]], [B_smax])
                        P.op("vector", (lambda bk, pv3: (lambda e: e.tensor_tensor(
                            out=Ef[:, bk * 4:bk * 4 + 4, :], in0=pv3,
                            in1=smax[:, bk * 4:bk * 4 + 4].unsqueeze(2).to_broadcast([128, 4, 128]),
                            op=ALU.subtract)))(bk, pv3), [psyB[bk], B_smax], [B_Ef])
                    P.op("scalar", lambda e: e.activation(out=Ebt[:], in_=Ef[:], func=AF.Exp), [B_Ef], [B_Ebt])
                    P.op("vector", lambda e: e.tensor_copy(out=Ef[:], in_=Ebt[:]), [B_Ebt], [B_Ef])
                    Eb4 = Ebt[:].rearrange("p (h two) n -> p h two n", two=2)
                    Ef4 = Ef[:].rearrange("p (h two) n -> p h two n", two=2)
                    P.op("gpsimd", (lambda tt, Eb4: (lambda e: e.tensor_copy(out=E1b[:, tt, :, :], in_=Eb4[:, :, 1, :])))(tt, Eb4),
                         [B_Ebt], [B_E1[tt]])
                    B_topg = P.bufs(16, "topg")
                    B_wkg = P.bufs(16, "wkg")
                    for g in range(16):
                        P.op("vector", (lambda g: (lambda e: e.max(out=top[:, g, 0:8], in_=Ef[:, g, :])))(g),
                             [B_Ef, B_top], [B_topg[g]])
                    for g in range(16):
                        P.op("vector", (lambda g: (lambda e: e.match_replace(
                            out=wk[:, g, :], in_to_replace=top[:, g, 0:8], in_values=Ef[:, g, :], imm_value=NEG)))(g),
                            [B_Ef, B_topg[g], B_wk], [B_wkg[g]])
                    for g in range(16):
                        P.op("vector", (lambda g: (lambda e: e.max(out=top[:, g, 8:16], in_=wk[:, g, :])))(g),
                             [B_wkg[g]], [B_topg[g]])
                    top4 = top[:].rearrange("p (h two) k -> p h two k", two=2)
                    P.op("vector", lambda e: e.tensor_tensor(
                        out=cand[:].rearrange("p h (i j) -> p h i j", i=16),
                        in0=top4[:, :, 0, :].unsqueeze(3).to_broadcast([128, 8, 16, 16]),
                        in1=top4[:, :, 1, :].unsqueeze(2).to_broadcast([128, 8, 16, 16]), op=ALU.mult),
                        B_topg, [B_cand, B_top])
                    wk8 = wk[:].rearrange("p (h a) b -> p h (a b)", a=2)
                    B_ctoph = P.bufs(8, "ctoph")
                    B_wkh = P.bufs(8, "wkh")
                    for h in range(8):
                        P.op("vector", (lambda h: (lambda e: e.max(out=ctop[:, h, 0:8], in_=cand[:, h, :])))(h),
                             [B_cand, B_ctop], [B_ctoph[h]])
                    for h in range(8):
                        P.op("vector", (lambda h, wk8: (lambda e: e.match_replace(
                            out=wk8[:, h, :], in_to_replace=ctop[:, h, 0:8], in_values=cand[:, h, :], imm_value=NEG)))(h, wk8),
                            [B_cand, B_ctoph[h]] + B_wkg, [B_wkh[h]])
                    for h in range(8):
                        P.op("vector", (lambda h, wk8: (lambda e: e.max(out=ctop[:, h, 8:16], in_=wk8[:, h, :])))(h, wk8),
                             [B_wkh[h]], [B_ctoph[h]])
                    B_ctop_all = B_ctoph
                    P.op("vector", lambda e: e.reciprocal(out=zz[:, 24:32], in_=ctop[:, :, 15]), B_ctop_all, [B_zz, B_ctop, B_wk])
                    P.op("vector", (lambda tt, Ef4: (lambda e: e.tensor_tensor(
                        out=E0p[:, tt, :, :], in0=Ef4[:, :, 0, :],
                        in1=zz[:, 24:32].unsqueeze(2).to_broadcast([128, 8, 128]), op=ALU.mult)))(tt, Ef4),
                        [B_Ef, B_zz], [B_E0[tt]])
                    P.op("vector", lambda e: e.tensor_reduce(out=zz[:, 0:8], in_=ctop[:], axis=AX.X, op=ALU.add),
                         B_ctop_all, [B_zz])
                    P.op("vector", lambda e: e.reciprocal(out=zz[:, 8:16], in_=zz[:, 0:8]), [B_zz], [B_zz])
                    P.op("vector", lambda e: e.tensor_tensor(out=zz[:, 16:24], in0=zz[:, 8:16], in1=ctop[:, :, 15],
                                                             op=ALU.mult), [B_zz] + B_ctop_all, [B_zz])
                    for h in range(8):
                        P.op("gpsimd", (lambda tt, h: (lambda e: e.tensor_scalar(
                            out=Dm[:, tt, h, :], in0=identf[:], scalar1=zz[:, 16 + h:17 + h], scalar2=None,
                            op0=ALU.mult)))(tt, h), [B_zz, B_const], [B_Dm[tt]])
                return B_hT, B_E1, B_E0, B_Dm

            def peer_main(blk, B_hT, B_E1, B_E0, B_Dm):
                utb, vb, ge, AT, Pf = m_utb, m_vb, m_ge, m_AT, m_Pf
                B_utb = P.bufs(6, "utb")
                B_vb = P.bufs(8, "vb")
                B_ge = P.bufs(2, "ge")
                B_AT = P.bufs(8, "AT")
                B_Pf = P.bufs(3, "Pf")
                B_M = [P.bufs(NTT, "M0_"), P.bufs(NTT, "M1_")]
                B_yacc = P.bufs(NTT, "yacc")
                pfc = [0]

                def stageA(sc):
                    mv = Mv[sc % 2]
                    for tt in range(NTT):
                        pf = Pf[pfc[0] % 3]
                        B_pf = B_Pf[pfc[0] % 3]
                        pfc[0] += 1
                        if tt % 2 == 0:
                            P.op("gpsimd", (lambda pf, tt, sc: (lambda e: e.tensor_tensor(
                                out=pf[:],
                                in0=E0p[:, tt, :, 2 * sc:2 * sc + 2].unsqueeze(3).to_broadcast([128, 8, 2, 128]),
                                in1=E1b[:, tt, :, :].unsqueeze(2).to_broadcast([128, 8, 2, 128]), op=ALU.mult)))(pf, tt, sc),
                                [B_E0[tt], B_E1[tt]], [B_pf])
                        else:
                            def pfn(e, pf=pf, tt=tt, sc=sc):
                                ins = None
                                for h in range(8):
                                    for a in range(2):
                                        ins = e.activation(out=pf[:, h, a, :], in_=E1b[:, tt, h, :], func=AF.Copy,
                                                           scale=E0p[:, tt, h, 2 * sc + a:2 * sc + a + 1])
                                return ins
                            P.op("scalar", pfn, [B_E0[tt], B_E1[tt]], [B_pf])
                        P.op("vector", (lambda pf, tt, mv: (lambda e: e.scalar_tensor_tensor(
                            out=mv[:, tt, :, :, :], in0=pf[:], scalar=THR, in1=pf[:],
                            op0=ALU.is_ge, op1=ALU.mult)))(pf, tt, mv), [B_pf], [B_M[sc % 2][tt]])

                def stageDU(sc):
                    for k in range(2):
                        et = 2 * sc + k
                        P.dma((lambda et: (lambda e: e.dma_start(out=utb[et % 6][:], in_=UT_d[et])))(et),
                              f"d_utb{et % 6}", writes=[B_utb[et % 6]], eng="gpsimd")

                def stageDV(sc):
                    for k in range(2):
                        et = 2 * sc + k
                        P.dma((lambda et: (lambda e: e.dma_start(out=vb[et % 8][:], in_=V_d[et])))(et),
                              f"d_vb{et % 8}", writes=[B_vb[et % 8]], eng="gpsimd")

                def stageB(sc):
                    mv = Mv[sc % 2]
                    for k in range(2):
                        et = 2 * sc + k
                        s4 = et % 8
                        u6 = et % 6
                        mm_group(P, ps[k][:, :], [(utb[u6][:, dc, :], hT[:, dc, :]) for dc in range(16)],
                                 [B_utb[u6], B_hT], [psB[k]])

                        def gfn(e, k=k, mv=mv):
                            ins = None
                            for tt in range(NTT):
                                for h in range(8):
                                    ins = e.matmul(ps[2 + k][:, tt * 128:(tt + 1) * 128], lhsT=mv[:, tt, h, k, :],
                                                   rhs=Dm[:, tt, h, :], start=(h == 0), stop=(h == 7))
                            return ins
                        P.op("tensor", gfn, B_M[sc % 2] + B_Dm, [psB[2 + k]])
                        P.op("scalar", (lambda k: (lambda e: e.activation(out=ge[k][:], in_=ps[k][:, :], func=AF.Gelu)))(k),
                             [psB[k]], [B_ge[k]])
                        P.op("vector", (lambda k, s4: (lambda e: e.tensor_tensor(out=AT[s4][:], in0=ps[2 + k][:, :],
                                                                                 in1=ge[k][:], op=ALU.mult)))(k, s4),
                             [psB[2 + k], B_ge[k]], [B_AT[s4]])

                def stageC(j):
                    et0 = 4 * j
                    sl = [(et0 + k) % 8 for k in range(4)]
                    for tt in range(NTT):
                        for hf in range(2):
                            def yfn(e, tt=tt, hf=hf, sl=sl):
                                ins = None
                                for n in (2 * hf, 2 * hf + 1):
                                    for k in range(4):
                                        ins = e.matmul(psyb(n), lhsT=AT[sl[k]][:, tt * 128:(tt + 1) * 128],
                                                       rhs=vb[sl[k]][:, n * 512:(n + 1) * 512], start=(k == 0),
                                                       stop=(k == 3))
                                return ins
                            pb = [psyB[2 * hf], psyB[2 * hf + 1]]
                            P.op("tensor", yfn, [B_AT[i] for i in sl] + [B_vb[i] for i in sl], pb)
                            cs_ = slice(hf * 1024, (hf + 1) * 1024)
                            if j == 0:
                                P.op("vector", (lambda tt, cs_: (lambda e: e.tensor_copy(out=yacc[:, tt, cs_],
                                                                                         in_=psy[:, cs_])))(tt, cs_),
                                     pb, [B_yacc[tt]])
                            else:
                                P.op("vector", (lambda tt, cs_: (lambda e: e.tensor_tensor(
                                    out=yacc[:, tt, cs_], in0=psy[:, cs_], in1=yacc[:, tt, cs_], op=ALU.add)))(tt, cs_),
                                    pb + [B_yacc[tt]], [B_yacc[tt]])

                NSC = 64
                stageDU(0)
                stageDU(1)
                stageDV(0)
                stageA(0)
                for sc in range(NSC):
                    if sc + 1 < NSC:
                        stageA(sc + 1)
                    stageB(sc)
                    if sc + 2 < NSC:
                        stageDU(sc + 2)
                    if sc + 1 < NSC:
                        stageDV(sc + 1)
                    if sc % 2 == 0 and sc >= 2:
                        stageC((sc - 2) // 2)
                stageC(NSC // 2 - 1)
                return B_yacc

            def peer_epilogue(blk, B_yacc):
                t0 = blk * TB
                xtmp, g2bc, fnbc = e_xtmp, e_g2bc, e_fnbc
                B_xe = P.bufs(4, "xe")
                B_bc = P.buf("bc2")
                B_junk2 = P.bufs(2, "junk")
                B_st2 = P.bufs(2, "state")
                B_out = P.buf("out")
                P.dma(lambda e: e.dma_start(out=g2bc[:], in_=g2s_d.partition_broadcast(128)), "d_bc2", writes=[B_bc],
                      eng="scalar")
                P.dma(lambda e: e.dma_start(out=fnbc[:], in_=fnorm_d.partition_broadcast(128)), "d_bc2", writes=[B_bc],
                      eng="scalar")

                def ep_load(tt, s):
                    P.dma(lambda e: e.dma_start(out=xtmp[s][:], in_=x1s_d[t0 + tt * 128:t0 + (tt + 1) * 128, :]),
                          f"d_xe{s}", writes=[B_xe[s]])
                for tt in range(NTT):
                    ep_load(tt, tt)

                def ep_tt(tt, s, junk, stat, B_junk, B_st):
                    P.op("gpsimd", lambda e: e.tensor_tensor(out=yacc[:, tt, :], in0=yacc[:, tt, :], in1=g2bc[:],
                                                             op=ALU.mult), [B_yacc[tt], B_bc], [B_yacc[tt]])
                    P.op("vector", lambda e: e.tensor_tensor(out=xtmp[s][:], in0=xtmp[s][:], in1=yacc[:, tt, :],
                                                             op=ALU.add), [B_xe[s], B_yacc[tt]], [B_xe[s]])
                    P.op("scalar", lambda e: e.activation(out=junk[:], in_=xtmp[s][:], func=AF.Square),
                         [B_xe[s]], [B_junk])
                    P.op("vector", lambda e: e.tensor_reduce(out=stat[:, 0:1], in_=junk[:], axis=AX.X, op=ALU.add),
                         [B_junk], [B_st])
                    P.op("scalar", lambda e: e.activation(out=stat[:, 1:2], in_=stat[:, 0:1], func=AF.Ln,
                                                          scale=1.0 / D, bias=epsc[:, 0:1]), [B_st, B_const], [B_st])
                    P.op("scalar", lambda e: e.activation(out=stat[:, 2:3], in_=stat[:, 1:2], func=AF.Exp, scale=-0.5),
                         [B_st], [B_st])
                    P.op("vector", lambda e: e.scalar_tensor_tensor(
                        out=xtmp[s][:], in0=xtmp[s][:], scalar=stat[:, 2:3], in1=fnbc[:], op0=ALU.mult,
                        op1=ALU.mult), [B_xe[s], B_st, B_bc], [B_xe[s]])
                    P.dma(lambda e: e.dma_start(out=out_d[t0 + tt * 128:t0 + (tt + 1) * 128, :], in_=xtmp[s][:]),
                          f"d_out{s}", reads=[B_xe[s]], writes=[B_out])
                for tt in range(NTT):
                    ep_tt(tt, tt, e_junk[tt % 2], e_stat[tt % 2], B_junk2[tt % 2], B_st2[tt % 2])
                return B_out

            outs = []
            for blk in range(NBLK):
                P.fence()
                r = peer_prologue(blk)
                P.fence()
                if debug == "p2a":
                    dbg2 = nc.dram_tensor("dbg2", [128, NTT * 8 * 128 * 3], F32, kind="ExternalOutput").ap()
                    cv = sb("cv", [128, NTT * 8 * 128])
                    B_cv = P.buf("cv")
                    B_o = P.buf("dbgo")
                    n1 = NTT * 8 * 128
                    P.dma(lambda e: e.dma_start(out=dbg2[:, 0:n1], in_=E0p[:].rearrange("p a b c -> p (a b c)")), "d_dbg2",
                          reads=r[2], writes=[B_o])
                    P.op("vector", lambda e: e.tensor_copy(out=cv[:], in_=E1b[:].rearrange("p a b c -> p (a b c)")),
                         r[1], [B_cv])
                    P.dma(lambda e: e.dma_start(out=dbg2[:, n1:2 * n1], in_=cv[:]), "d_dbg2", reads=[B_cv], writes=[B_o])
                    cv2 = sb("cv2", [128, NTT * 8 * 128])
                    B_cv2 = P.buf("cv2")
                    P.op("vector", lambda e: e.tensor_copy(out=cv2[:], in_=Dm[:].rearrange("p a b c -> p (a b c)")),
                         r[3], [B_cv2])
                    P.dma(lambda e: e.dma_start(out=dbg2[:, 2 * n1:3 * n1], in_=cv2[:]), "d_dbg2", reads=[B_cv2], writes=[B_o])
                    P.wait_all("sync", [B_o])
                    P.emit("main")
                    return nc
                B_yacc = peer_main(blk, *r)
                P.fence()
                if debug == "p2m":
                    dbg3 = nc.dram_tensor("dbg3", [128, NTT * D], F32, kind="ExternalOutput").ap()
                    B_o = P.buf("dbgo")
                    P.dma(lambda e: e.dma_start(out=dbg3, in_=yacc[:].rearrange("p a b -> p (a b)")), "d_dbg3",
                          reads=B_yacc, writes=[B_o])
                    P.wait_all("sync", [B_o])
                    P.emit("main")
                    return nc
                outs.append(peer_epilogue(blk, B_yacc))
                if debug == "p2e" and blk == 1:
                    break
            P.wait_all("sync", outs)
        P.emit("main")
    return nc


def _tile_w(w):
    K, N = w.shape
    return np.ascontiguousarray(w.reshape(16, 128, N // 128, 128).transpose(2, 1, 0, 3))


def _cols(v):
    return np.ascontiguousarray(np.asarray(v, np.float32).reshape(-1, 128).T)


def prep_inputs(x, c, w_ada, b_ada, norm1, w_in, pool_w, pool_scale, lb_logits, hg_norm, w_out, norm2, peer_wq,
                peer_keys, peer_u, peer_v, final_norm):
    f = lambda a: np.asarray(a, np.float32)
    sh = {}
    wa = f(w_ada)[0]
    sh["wada"] = np.ascontiguousarray(wa.reshape(16, 128, 24, 512).transpose(2, 1, 0, 3))
    sh["bada"] = _cols(f(b_ada)[0])
    sh["pvec"] = np.ascontiguousarray(np.stack([_cols(f(norm1)[0]), _cols(f(norm2)[0]), _cols(f(pool_scale)[0]),
                                                _cols(f(hg_norm)[0].reshape(-1)), _cols(f(lb_logits)[0]),
                                                _cols(f(lb_logits)[1])], axis=1))
    sh["fnorm"] = np.ascontiguousarray(f(final_norm))
    sh["win"] = _tile_w(f(w_in)[0])
    sh["poolw"] = np.ascontiguousarray(f(pool_w)[0].reshape(4, 2, 128, 512).transpose(2, 0, 1, 3).reshape(128, 8, 512))
    sh["wout"] = _tile_w(f(w_out)[0])
    sh["wq"] = _tile_w(f(peer_wq)[0])
    sh["keysT"] = np.ascontiguousarray(f(peer_keys)[0].transpose(3, 0, 1, 2).reshape(128, 16, 128))
    sh["UT"] = np.ascontiguousarray(f(peer_u)[0].reshape(128, 128, 16, 128).transpose(0, 3, 2, 1))
    sh["V"] = np.ascontiguousarray(f(peer_v)[0].reshape(128, 128, D))
    sh["ident"] = np.eye(128, dtype=np.float32)
    sh["utm"] = np.triu(np.ones((128, 128), np.float32))
    pinv = np.zeros((128, 4, 16), np.float32)
    for g, w in enumerate((2, 4, 8, 16)):
        pinv[:, g, :] = 1.0 / np.minimum(np.arange(16) + 1, w).astype(np.float32)[None, :]
    sh["pinv"] = pinv
    xs = f(x)
    cs = f(c)
    maps = []
    for b in range(xs.shape[0]):
        m = dict(sh)
        m["x"] = np.ascontiguousarray(xs[b])
        m["cT"] = _cols(cs[b])
        maps.append(m)
    return maps


def kernel(**inputs):
    maps = prep_inputs(**inputs)
    nc = build_program()
    res = run_bass_kernel_spmd(nc, maps, core_ids=list(range(len(maps))))
    out = np.stack([np.asarray(r["out"], np.float32) for r in res.results], axis=0)
    return out
```

```python
from contextlib import ExitStack
import numpy as np
import ml_dtypes
import concourse.bass as bass
import concourse.mybir as mybir
from concourse.bass_utils import run_bass_kernel_spmd

F32 = mybir.dt.float32
BF16 = mybir.dt.bfloat16
AF = mybir.ActivationFunctionType
ALU = mybir.AluOpType
AX = mybir.AxisListType

D = 2048
SEQ = 2048
TB = 512
NBLK = SEQ // TB
NTT = TB // 128
EPS = 1e-6
NEG = -1.0e30
SAME_ENGINE_RAW = True


class Buf:
    __slots__ = ("name", "w", "r")

    def __init__(self, name):
        self.name = name
        self.w = {}
        self.r = {}


class Prog:
    ENGS = ("sync", "scalar", "vector", "gpsimd", "tensor")

    def __init__(self, nc, stack):
        self.nc = nc
        self.stack = stack
        self.sems = {}
        self.cnt = {}
        self.known = {e: {} for e in self.ENGS}
        self.ops = []
        self.nbuf = 0
        self.base = {}
        for e in self.ENGS[1:]:
            self._sem("E_" + e)

    def _sem(self, key):
        if key not in self.sems:
            self.sems[key] = self.stack.enter_context(self.nc.semaphore(key))
            self.cnt[key] = 0
        return self.sems[key]

    def buf(self, name=None):
        self.nbuf += 1
        b = Buf(name or f"b{self.nbuf}")
        b.w = dict(self.base)
        return b

    def fence(self):
        self.base = {k: v for k, v in self.cnt.items() if v > 0}

    def bufs(self, n, name="b"):
        return [self.buf(f"{name}{i}") for i in range(n)]

    def _collect(self, eng, reads, writes):
        need = {}
        own = "E_" + eng

        def add(d, raw):
            for k, v in d.items():
                if k == own and (eng == "tensor" or not SAME_ENGINE_RAW):
                    continue
                if need.get(k, 0) < v:
                    need[k] = v

        for b in reads:
            add(b.w, True)
        for b in writes:
            add(b.w, False)
            add(b.r, False)
        kn = self.known[eng]
        waits = []
        for k, v in need.items():
            if kn.get(k, 0) >= v:
                continue
            kn[k] = v
            waits.append((k, v))
        return waits

    def op(self, eng, fn, reads=(), writes=()):
        waits = self._collect(eng, reads, writes)
        key = "E_" + eng
        self.cnt[key] += 1
        v = self.cnt[key]
        for b in reads:
            if b.r.get(key, 0) < v:
                b.r[key] = v
        for b in writes:
            if b.w.get(key, 0) < v:
                b.w[key] = v
        self.ops.append((eng, fn, waits, (key, 1)))

    def dma(self, fn, semkey, reads=(), writes=(), eng="sync"):
        self._sem(semkey)
        waits = self._collect(eng, reads, writes)
        self.cnt[semkey] += 16
        v = self.cnt[semkey]
        for b in reads:
            if b.r.get(semkey, 0) < v:
                b.r[semkey] = v
        for b in writes:
            if b.w.get(semkey, 0) < v:
                b.w[semkey] = v
        self.ops.append((eng, fn, waits, (semkey, 16)))

    def wait_all(self, eng, bufs, also_reads=False):
        need = []
        if also_reads:
            waits = self._collect(eng, (), bufs)
        else:
            waits = self._collect(eng, bufs, ())
        self.ops.append((eng, None, waits, None))

    def emit(self, name=None):
        nc = self.nc
        ops = self.ops
        sems = self.sems
        import bisect
        waited = {}
        for (_, _, waits, _) in ops:
            for (k, v) in waits:
                if k.startswith("E_"):
                    waited.setdefault(k, set()).add(v)
        waited = {k: sorted(vs) for k, vs in waited.items()}
        base = getattr(self, "_rank_base", {})

        def rank(k, v):
            return base.get(k, 0) + bisect.bisect_right(waited.get(k, []), v)

        seqs = getattr(self, "_seq_base", {})
        plan = []
        cur = dict(seqs)
        for (eng, fn, waits, inc) in ops:
            w2 = []
            for (k, v) in waits:
                if k.startswith("E_"):
                    w2.append((k, rank(k, v)))
                else:
                    w2.append((k, v))
            sig = None
            if inc is not None:
                k, amt = inc
                if k.startswith("E_"):
                    cur[k] = cur.get(k, 0) + 1
                    n = cur[k]
                    lst = waited.get(k, [])
                    i = bisect.bisect_left(lst, n)
                    if i < len(lst) and lst[i] == n:
                        sig = (k, 1)
                else:
                    sig = (k, amt)
            plan.append((eng, fn, w2, sig))
        with nc.Block(name) as blk:
            for e in self.ENGS:
                mine = [o for o in plan if o[0] == e]
                if not mine:
                    continue

                def body(eng, mine=mine):
                    for (_, fn, waits, sig) in mine:
                        for (k, v) in waits:
                            eng.wait_ge(sems[k], v)
                        if fn is None:
                            continue
                        ins = fn(eng)
                        if sig is not None:
                            ins.then_inc(sems[sig[0]], sig[1])

                getattr(blk, e)(body)
        nsig = sum(1 for o in plan if o[3] is not None)
        self.stats = (len(plan), nsig)
        self.ops = []


def mm_group(P, out_ap, pairs, reads, writes):
    pairs = list(pairs)

    def fn(e):
        n = len(pairs)
        ins = None
        for i, (l, r) in enumerate(pairs):
            ins = e.matmul(out_ap, lhsT=l, rhs=r, start=(i == 0), stop=(i == n - 1))
        return ins

    P.op("tensor", fn, reads, writes)


def build_program(debug=None):
    nc = bass.Bass("TRN2", target_bir_lowering=False)
    dt_in = lambda n, s: nc.dram_tensor(n, s, F32, kind="ExternalInput").ap()
    x_d = dt_in("x", [SEQ, D])
    cT_d = dt_in("cT", [128, 16])
    wada_d = dt_in("wada", [24, 128, 16, 512])
    bada_d = dt_in("bada", [128, 96])
    pvec_d = dt_in("pvec", [128, 6, 16])
    fnorm_d = dt_in("fnorm", [D])
    win_d = dt_in("win", [104, 128, 16, 128])
    poolw_d = dt_in("poolw", [128, 8, 512])
    wout_d = dt_in("wout", [16, 128, 16, 128])
    wq_d = dt_in("wq", [16, 128, 16, 128])
    keysT_d = dt_in("keysT", [128, 16, 128])
    UT_d = dt_in("UT", [128, 128, 16, 128])
    V_d = dt_in("V", [128, 128, D])
    ident_d = dt_in("ident", [128, 128])
    utm_d = dt_in("utm", [128, 128])
    pinv_d = dt_in("pinv", [128, 4, 16])
    out_d = nc.dram_tensor("out", [SEQ, D], F32, kind="ExternalOutput").ap()
    x1_kind = "ExternalOutput" if debug in ("p1", "zero") else "Internal"
    x1s_d = nc.dram_tensor("x1s", [SEQ, D], F32, kind=x1_kind).ap()
    g1s_d = nc.dram_tensor("g1s", [D], F32, kind=x1_kind).ap()
    g2s_d = nc.dram_tensor("g2s", [D], F32, kind=x1_kind).ap()
    dbg_d = None
    if debug == "p0":
        dbg_d = nc.dram_tensor("dbg", [128, 160], F32, kind="ExternalOutput").ap()

    with ExitStack() as gs:
        P = Prog(nc, gs)

        uid = [0]

        def mk_alloc(stack):
            def sb(name, shape, dt=F32):
                uid[0] += 1
                return stack.enter_context(nc.sbuf_tensor(f"{name}_{uid[0]}", shape, dt))
            return sb

        gsb = mk_alloc(gs)
        identf = gsb("identf", [128, 128])
        identb = gsb("identb", [128, 128], BF16)
        utm = gsb("utm_sb", [128, 128])
        onesf = gsb("onesf", [128, 128])
        onesb = gsb("onesb", [128, 128], BF16)
        pinv = gsb("pinv_sb", [128, 4, 16])
        pv = gsb("pv", [128, 6, 16])
        par = gsb("par", [128, 12, 16])
        A1, SH1, G1C, A2, SH2, G2C, PSC, HGN, LB, OML = range(10)
        epsc = gsb("epsc", [128, 1])
        ps = [gs.enter_context(nc.psum_tensor(f"ps{i}", [128, 512], F32)) for i in range(4)]
        psy = gs.enter_context(nc.psum_tensor("psy", [128, 2048], F32))
        psB = [P.buf(f"ps{i}") for i in range(4)]
        psyB = [P.buf(f"psy{i}") for i in range(4)]

        def psyb(i):
            return psy[:, i * 512:(i + 1) * 512]

        B_const = P.buf("consts")
        B_par = P.buf("par")

        with ExitStack() as s0:
            sb = mk_alloc(s0)
            NWA = 4
            wa = [sb(f"wa{i}", [128, 16, 512], BF16) for i in range(NWA)]
            waB = P.bufs(NWA, "wa")
            csb = sb("csb", [128, 16], BF16)
            cst = sb("cst", [128, 16])
            cs = sb("cs", [128, 16])
            bada = sb("bada_sb", [128, 96])
            ada = sb("ada_sb", [128, 96])
            tmpc = sb("tmpc", [128, 16])
            B_c = P.buf("c")
            B_ada = P.buf("ada")
            for (dst, src) in ((identf, ident_d), (utm, utm_d), (pinv, pinv_d), (pv, pvec_d),
                               (cst, cT_d), (bada, bada_d)):
                P.dma((lambda d, s: (lambda e: e.dma_start(out=d[:], in_=s)))(dst, src), "d_const",
                      writes=[B_const])
            P.op("vector", lambda e: e.tensor_copy(out=identb[:], in_=identf[:]), [B_const], [B_const])
            P.op("gpsimd", lambda e: e.memset(onesf[:], 1.0), [], [B_const])
            P.op("gpsimd", lambda e: e.memset(onesb[:], 1.0), [], [B_const])
            P.op("gpsimd", lambda e: e.memset(epsc[:], EPS), [], [B_const])
            P.op("scalar", lambda e: e.activation(out=cs[:], in_=cst[:], func=AF.Silu), [B_const], [B_c])
            P.op("vector", lambda e: e.tensor_copy(out=csb[:], in_=cs[:]), [B_c], [B_c])
            for j in range(24):
                P.dma((lambda j: (lambda e: e.dma_start(out=wa[j % NWA][:], in_=wada_d[j])))(j), f"d_wa{j % NWA}",
                      writes=[waB[j % NWA]], eng="gpsimd")

                def fn(e, j=j):
                    ins = None
                    for q in range(4):
                        col = j * 4 + q
                        for kc in range(16):
                            ins = e.matmul(ps[0][:, col:col + 1], lhsT=wa[j % NWA][:, kc, q * 128:(q + 1) * 128],
                                           rhs=csb[:, kc:kc + 1], start=(kc == 0), stop=(kc == 15))
                    return ins
                P.op("tensor", fn, [waB[j % NWA], B_c], [psB[0]])
            P.op("vector", lambda e: e.tensor_tensor(out=ada[:], in0=ps[0][:, 0:96], in1=bada[:], op=ALU.add),
                 [psB[0], B_const], [B_ada])
            P.op("vector", lambda e: e.scalar_tensor_tensor(out=par[:, A1, :], in0=ada[:, 16:32], scalar=1.0,
                                                            in1=pv[:, 0, :], op0=ALU.add, op1=ALU.mult),
                 [B_ada, B_const], [B_par])
            P.op("vector", lambda e: e.tensor_copy(out=par[:, SH1, :], in_=ada[:, 0:16]), [B_ada], [B_par])
            P.op("vector", lambda e: e.tensor_scalar(out=par[:, G1C, :], in0=ada[:, 32:48], scalar1=1.0,
                                                     scalar2=None, op0=ALU.add), [B_ada], [B_par])
            P.op("vector", lambda e: e.scalar_tensor_tensor(out=par[:, A2, :], in0=ada[:, 64:80], scalar=1.0,
                                                            in1=pv[:, 1, :], op0=ALU.add, op1=ALU.mult),
                 [B_ada, B_const], [B_par])
            P.op("vector", lambda e: e.tensor_copy(out=par[:, SH2, :], in_=ada[:, 48:64]), [B_ada], [B_par])
            P.op("vector", lambda e: e.tensor_scalar(out=par[:, G2C, :], in0=ada[:, 80:96], scalar1=1.0,
                                                     scalar2=None, op0=ALU.add), [B_ada], [B_par])
            P.op("vector", lambda e: e.tensor_copy(out=par[:, PSC, :], in_=pv[:, 2, :]), [B_const], [B_par])
            P.op("vector", lambda e: e.tensor_copy(out=par[:, HGN, :], in_=pv[:, 3, :]), [B_const], [B_par])
            B_tc = P.buf("tmpc")
            P.op("vector", lambda e: e.tensor_tensor(out=tmpc[:], in0=pv[:, 4, :], in1=pv[:, 5, :], op=ALU.subtract),
                 [B_const], [B_tc])
            P.op("scalar", lambda e: e.activation(out=par[:, LB, :], in_=tmpc[:], func=AF.Sigmoid), [B_tc], [B_par])
            P.op("vector", lambda e: e.tensor_scalar(out=par[:, OML, :], in0=par[:, LB, :], scalar1=-1.0, scalar2=1.0,
                                                     op0=ALU.mult, op1=ALU.add), [B_par], [B_par])
            B_gs = P.buf("gs")
            P.dma(lambda e: e.dma_start(out=g1s_d.rearrange("(j p) -> p j", p=128), in_=par[:, G1C, :],
                                        allow_slow_non_contiguous=True), "d_gs", reads=[B_par], writes=[B_gs])
            P.dma(lambda e: e.dma_start(out=g2s_d.rearrange("(j p) -> p j", p=128), in_=par[:, G2C, :],
                                        allow_slow_non_contiguous=True), "d_gs", reads=[B_par], writes=[B_gs])
            outs = [B_gs]
            if debug == "p0":
                B_dbg = P.buf("dbg")
                P.dma(lambda e: e.dma_start(out=dbg_d[:, 0:160], in_=par[:, 0:10, :].rearrange("p a b -> p (a b)")),
                      "d_dbg", reads=[B_par], writes=[B_dbg])
                outs.append(B_dbg)
            P.wait_all("sync", outs)
            P.fence()
        if debug == "p0":
            P.emit("main")
            return nc

        with ExitStack() as s1:
            sb = mk_alloc(s1)
            xt = sb("xt", [128, NTT, D])
            hT = sb("hT", [128, 16, TB], BF16)
            mT = sb("mT", [128, 16, TB], BF16)
            pe = sb("pe", [128, 8, 16 + TB])
            pooledT = sb("pooledT", [128, 8, TB], BF16)
            Sst = sb("Sst", [128, 16, 128])
            g1bc = sb("g1bc", [128, D])
            NW = 6
            PF = 4
            wbf = [sb(f"wbf{i}", [128, 16, 128], BF16) for i in range(NW)]
            poolw_b = sb("poolw_b", [128, 8, 512], BF16)
            psT = ps[3][:].bitcast(BF16)
            W = {}
            WB = {}
            alias = {"sig": 0, "f": 0, "lf": 1, "kk": 2, "cum": 3, "cumref": 4, "eq": 5, "ek": 6,
                     "sd": 7, "rstd": 7, "t1": 8, "t2": 8, "t3": 8, "u1": 9}
            Tt = [sb(f"wT{i}", [128, TB]) for i in range(10)]
            TBf = [P.buf(f"wT{i}") for i in range(10)]
            G3 = [[sb(f"g3_{i}_{j}", [128, TB], BF16) for j in range(3)] for i in range(2)]
            B_G3 = [[P.buf(f"g3_{i}_{j}") for j in range(3)] for i in range(2)]
            qr = sb("qr", [128, TB])
            B_qr = P.buf("qr")
            for n, i in alias.items():
                W[n] = Tt[i]
                WB[n] = TBf[i]
            qtb = sb("qtb", [128, TB], BF16)
            ktb = sb("ktb", [128, TB], BF16)
            iTb = sb("iTb", [128, TB], BF16)
            osqb = sb("osqb", [128, TB], BF16)
            tok = sb("tok", [128, 8, 128], BF16)
            attm = [sb(f"attm{i}", [128, 128], BF16) for i in range(4)]
            Sp = [sb(f"Sp{i}", [128, 128], BF16) for i in range(2)]
            tmpKI = [sb(f"tmpKI{i}", [128, 128]) for i in range(4)]
            esc = sb("esc", [128, 12])
            xnb = [sb(f"xn{i}", [128, D]) for i in range(2)]
            xn = xnb[0]
            statb = [sb(f"stat{i}", [128, 4]) for i in range(2)]
            pA = [sb(f"pA{i}", [128, 16 + TB]) for i in range(2)]
            o2t = sb("o2t", [128, NTT, 128])
            B_qtb, B_ktb, B_iTb, B_osqb, B_tok, B_esc, B_xn, B_stat, B_o2t = (
                P.buf(n) for n in ("qtb", "ktb", "iTb", "osqb", "tok", "esc", "xn", "stat", "o2t"))
            B_attm = P.bufs(4, "attm")
            B_Sp = P.bufs(2, "Sp")
            B_tmpKI = P.bufs(4, "tmpKI")
            B_pA = P.bufs(2, "pA")
            B_xt = P.bufs(NTT, "xt")
            B_hT = P.buf("hT")
            B_mT = P.buf("mT")
            B_pe = P.bufs(8, "pe")
            B_pooledT = P.buf("pooledT")
            B_S = P.bufs(16, "S")
            B_g1bc = P.buf("g1bc")
            B_wbf = P.bufs(NW, "wbf")
            B_poolw = P.buf("poolw")
            B_x1s = P.buf("x1s")
            wctr = [0]
            pctr = [0]

            blk_srcs = ([("win", c) for c in range(8)]
                        + [("win", base + hd) for hd in range(16) for base in (24, 88, 72, 56, 8, 40)]
                        + [("wout", n) for n in range(16)])
            all_srcs = blk_srcs * NBLK
            wiss = [0]

            def wstream(kind, idx):
                i = wctr[0]
                assert all_srcs[i] == (kind, idx), (i, all_srcs[i], kind, idx)
                while wiss[0] < min(len(all_srcs), i + 1 + PF):
                    j = wiss[0]
                    wiss[0] += 1
                    sj = j % NW
                    kd, ix = all_srcs[j]
                    src = win_d[ix] if kd == "win" else wout_d[ix]
                    P.dma((lambda sj, src: (lambda e: e.dma_start(out=wbf[sj][:], in_=src)))(sj, src), f"d_wbf{sj}",
                          writes=[B_wbf[sj]], eng="gpsimd")
                wctr[0] += 1
                s = i % NW
                return wbf[s], B_wbf[s]

            def proj(kind, idx, act_T, B_act):
                wb, wB = wstream(kind, idx)
                k = pctr[0] % 3
                pctr[0] += 1
                mm_group(P, ps[k][:, :], [(wb[:, kc, :], act_T[:, kc, :]) for kc in range(16)],
                         [wB, B_act], [psB[k]])
                return ps[k], psB[k]

            B_xnb = [B_xn, P.buf("xn1")]
            B_statb = [B_stat, P.buf("stat1")]

            def rms_to_T(tt, src, B_src, a_idx, sh_idx, dstT, B_dst):
                xn, B_xn, stat, B_stat = xnb[tt % 2], B_xnb[tt % 2], statb[tt % 2], B_statb[tt % 2]
                P.op("scalar", lambda e: e.activation(out=xn[:], in_=src, func=AF.Square), [B_src], [B_xn])
                P.op("vector", lambda e: e.tensor_reduce(out=stat[:, 0:1], in_=xn[:], axis=AX.X, op=ALU.add),
                     [B_xn], [B_stat])
                P.op("scalar", lambda e: e.activation(out=stat[:, 1:2], in_=stat[:, 0:1], func=AF.Ln,
                                                      scale=1.0 / D, bias=epsc[:, 0:1]), [B_stat, B_const], [B_stat])
                P.op("scalar", lambda e: e.activation(out=stat[:, 2:3], in_=stat[:, 1:2], func=AF.Exp, scale=-0.5),
                     [B_stat], [B_stat])
                P.op("vector", lambda e: e.tensor_scalar(out=xn[:], in0=src, scalar1=stat[:, 2:3], scalar2=None,
                                                         op0=ALU.mult), [B_src, B_stat], [B_xn])
                for gq in range(4):
                    k = pctr[0] % 3
                    pctr[0] += 1

                    def tfn(e, gq=gq, k=k):
                        ins = None
                        for j in range(4):
                            dc = gq * 4 + j
                            ins = e.transpose(out=ps[k][:, j * 128:(j + 1) * 128], in_=xn[:, dc * 128:(dc + 1) * 128],
                                              identity=identf[:])
                        return ins
                    P.op("tensor", tfn, [B_xn, B_const], [psB[k]])
                    for j in range(4):
                        dc = gq * 4 + j
                        o_ap = dstT[:, dc, tt * 128:(tt + 1) * 128]
                        i_ap = ps[k][:, j * 128:(j + 1) * 128]
                        if j % 2 == 0:
                            P.op("scalar", (lambda o_ap, i_ap, dc: (lambda e: e.activation(
                                out=o_ap, in_=i_ap, func=AF.Identity, scale=par[:, a_idx, dc:dc + 1],
                                bias=par[:, sh_idx, dc:dc + 1])))(o_ap, i_ap, dc), [psB[k], B_par], [B_dst])
                        else:
                            P.op("vector", (lambda o_ap, i_ap, dc: (lambda e: e.tensor_scalar(
                                out=o_ap, in0=i_ap, scalar1=par[:, a_idx, dc:dc + 1], scalar2=par[:, sh_idx, dc:dc + 1],
                                op0=ALU.mult, op1=ALU.add)))(o_ap, i_ap, dc), [psB[k], B_par], [B_dst])

            with ExitStack() as s1p:
                sbp = mk_alloc(s1p)
                xn4 = xn[:].rearrange("p (a b) -> p a b", a=4)
                for hf in range(2):
                    P.dma((lambda hf: (lambda e: e.dma_start(out=xn4, in_=poolw_d[:, hf * 4:hf * 4 + 4, :])))(hf), "d_pw",
                          writes=[B_xn])
                    P.op("vector", (lambda hf: (lambda e: e.tensor_copy(out=poolw_b[:, hf * 4:hf * 4 + 4, :], in_=xn4)))(hf),
                         [B_xn], [B_poolw])
                P.dma(lambda e: e.dma_start(out=g1bc[:], in_=g1s_d.partition_broadcast(128)), "d_g1bc",
                      writes=[B_g1bc])
                P.op("gpsimd", lambda e: e.memset(Sst[:], 0.0), [], B_S)
                P.op("gpsimd", lambda e: e.memset(pe[:], 0.0), [], B_pe)
                for i in range(2):
                    P.op("gpsimd", (lambda i: (lambda e: e.memset(pA[i][:], 0.0)))(i), [], [B_pA[i]])
                for i in range(4):
                    P.op("gpsimd", (lambda i: (lambda e: e.memset(attm[i][:], 0.0)))(i), [], [B_attm[i]])
                P.fence()

            for blk in range(NBLK):
                t0 = blk * TB
                for tt in range(NTT):
                    P.dma((lambda tt, t0: (lambda e: e.dma_start(out=xt[:, tt, :],
                                                                 in_=x_d[t0 + tt * 128:t0 + (tt + 1) * 128, :])))(tt, t0),
                          f"d_xt{tt}", writes=[B_xt[tt]])
                for tt in range(NTT):
                    rms_to_T(tt, xt[:, tt, :], B_xt[tt], A1, SH1, hT, B_hT)
                for c in range(8):
                    pp, pB = proj("win", c, hT, B_hT)
                    P.op("scalar", (lambda c, pp: (lambda e: e.copy(out=pe[:, c, 16:16 + TB], in_=pp[:, :])))(c, pp),
                         [pB], [B_pe[c]])
                    g = c // 2
                    w = (2, 4, 8, 16)[g]
                    cur = pe[:, c, :]
                    curB = B_pe[c]
                    sh = 1
                    i = 0
                    while sh < w:
                        dst = pA[i % 2]
                        dB = B_pA[i % 2]
                        P.op("gpsimd", (lambda dst, cur, sh: (lambda e: e.tensor_tensor(
                            out=dst[:, sh:16 + TB], in0=cur[:, sh:16 + TB], in1=cur[:, 0:16 + TB - sh], op=ALU.add)))(
                            dst, cur, sh), [curB], [dB])
                        cur = dst
                        curB = dB
                        sh *= 2
                        i += 1
                    P.op("vector", (lambda c, cur, w: (lambda e: e.scalar_tensor_tensor(
                        out=pooledT[:, c, :], in0=cur[:, 16:16 + TB], scalar=1.0 / w, in1=pe[:, c, 16:16 + TB],
                        op0=ALU.mult, op1=ALU.subtract)))(c, cur, w), [curB, B_pe[c]], [B_pooledT])
                    if blk == 0:
                        P.op("vector", (lambda c, cur, g: (lambda e: e.tensor_tensor(
                            out=cur[:, 16:32], in0=cur[:, 16:32], in1=pinv[:, g, :], op=ALU.mult)))(c, cur, g),
                            [curB, B_const], [curB])
                        P.op("vector", (lambda c, cur: (lambda e: e.tensor_tensor(
                            out=pooledT[:, c, 0:16], in0=cur[:, 16:32], in1=pe[:, c, 16:32], op=ALU.subtract)))(c, cur),
                            [curB, B_pe[c]], [B_pooledT])
                    P.op("gpsimd", (lambda c: (lambda e: e.tensor_copy(out=pe[:, c, 0:16], in_=pe[:, c, TB:TB + 16])))(c),
                         [B_pe[c]], [B_pe[c]])
                for hd in range(16):
                    par2 = hd % 2
                    sg, sbg, sa = G3[par2]
                    B_sg, B_sbg, B_sa = B_G3[par2]
                    def early(hd1, j):
                        tgt, B_tgt = ((W["sig"], WB["sig"]), (G3[hd1 % 2][1], B_G3[hd1 % 2][1]),
                                      (G3[hd1 % 2][2], B_G3[hd1 % 2][2]))[j]
                        pp, pB = proj("win", (24, 88, 72)[j] + hd1, hT, B_hT)
                        P.op("scalar", (lambda pp, tgt: (lambda e: e.activation(out=tgt[:], in_=pp[:, :],
                                                                                func=AF.Sigmoid)))(pp, tgt), [pB], [B_tgt])
                    if hd == 0:
                        for j in range(3):
                            early(0, j)
                    gp, gB = proj("win", 56 + hd, hT, B_hT)
                    P.op("scalar", (lambda gp, sg: (lambda e: e.activation(out=sg[:], in_=gp[:, :], func=AF.Silu)))(gp, sg),
                         [gB], [B_sg])
                    P.op("vector", (lambda hd: (lambda e: e.tensor_scalar(
                        out=W["f"][:], in0=W["sig"][:], scalar1=par[:, OML, hd:hd + 1], scalar2=par[:, LB, hd:hd + 1],
                        op0=ALU.mult, op1=ALU.add)))(hd), [WB["sig"], B_par], [WB["f"]])
                    P.op("scalar", lambda e: e.activation(out=W["lf"][:], in_=W["f"][:], func=AF.Ln),
                         [WB["f"]], [WB["lf"]])
                    P.op("gpsimd", lambda e: e.tensor_scalar(out=W["kk"][:], in0=W["f"][:], scalar1=-1.0, scalar2=1.0,
                                                             op0=ALU.mult, op1=ALU.add), [WB["f"]], [WB["kk"]])
                    for c in range(NTT):
                        P.op("vector", (lambda c: (lambda e: e.tensor_tensor_scan(
                            out=W["cum"][:, c * 128:(c + 1) * 128], data0=onesf[:], data1=W["lf"][:, c * 128:(c + 1) * 128],
                            initial=0.0, op0=ALU.mult, op1=ALU.add)))(c), [WB["lf"], B_const], [WB["cum"]])
                    qp, qB = proj("win", 8 + hd, hT, B_hT)
                    P.op("scalar", (lambda qp: (lambda e: e.copy(out=qr[:], in_=qp[:, :])))(qp), [qB], [B_qr])
                    ip, iB = proj("win", 40 + hd, hT, B_hT)
                    P.op("scalar", (lambda ip: (lambda e: e.copy(out=iTb[:], in_=ip[:, :])))(ip), [iB], [B_iTb])
                    cum3 = W["cum"][:].rearrange("p (c t) -> p c t", c=NTT)
                    cr3 = W["cumref"][:].rearrange("p (c t) -> p c t", c=NTT)
                    P.op("gpsimd", lambda e: e.tensor_tensor(out=cr3, in0=cum3,
                                                             in1=cum3[:, :, 63:64].to_broadcast([128, NTT, 128]),
                                                             op=ALU.subtract), [WB["cum"]], [WB["cumref"]])
                    P.op("scalar", lambda e: e.activation(out=W["eq"][:], in_=W["cumref"][:], func=AF.Exp),
                         [WB["cumref"]], [WB["eq"]])
                    P.op("scalar", lambda e: e.activation(out=W["ek"][:], in_=W["cumref"][:], func=AF.Exp, scale=-1.0),
                         [WB["cumref"]], [WB["ek"]])
                    P.op("scalar", lambda e: e.activation(out=esc[:, 0:4], in_=cum3[:, :, 63], func=AF.Exp),
                         [WB["cum"]], [B_esc])
                    P.op("scalar", lambda e: e.activation(out=esc[:, 4:8], in_=cum3[:, :, 127], func=AF.Exp),
                         [WB["cum"]], [B_esc])
                    P.op("scalar", lambda e: e.activation(out=esc[:, 8:12], in_=cr3[:, :, 127], func=AF.Exp),
                         [WB["cumref"]], [B_esc])
                    P.op("gpsimd", lambda e: e.tensor_tensor(out=ktb[:], in0=W["kk"][:], in1=W["ek"][:], op=ALU.mult),
                         [WB["kk"], WB["ek"]], [B_ktb])
                    P.op("vector", lambda e: e.tensor_tensor(out=qtb[:], in0=qr[:], in1=W["eq"][:], op=ALU.mult),
                         [B_qr, WB["eq"]], [B_qtb])

                    def tfn(e):
                        ins = None
                        for c in range(NTT):
                            ins = e.transpose(out=psT[:, c * 128:(c + 1) * 128], in_=iTb[:, c * 128:(c + 1) * 128],
                                              identity=identb[:])
                        for c in range(NTT):
                            ins = e.transpose(out=psT[:, (4 + c) * 128:(5 + c) * 128], in_=ktb[:, c * 128:(c + 1) * 128],
                                              identity=identb[:])
                        return ins
                    P.op("tensor", tfn, [B_iTb, B_ktb, B_const], [psB[3]])
                    P.op("vector", lambda e: e.tensor_copy(out=tok[:].rearrange("p a b -> p (a b)"), in_=psT[:, :]),
                         [psB[3]], [B_tok])
                    g = hd // 4
                    e0 = (hd % 4) * 128
                    kya = pctr[0] % 3
                    pctr[0] += 1
                    mm_group(P, ps[kya][:, :], [(poolw_b[:, g * 2 + kc, e0:e0 + 128], pooledT[:, g * 2 + kc, :]) for kc in range(2)],
                             [B_poolw, B_pooledT], [psB[kya]])
                    P.op("vector", (lambda hd, kya, sa: (lambda e: e.scalar_tensor_tensor(
                        out=W["u1"][:], in0=ps[kya][:, :], scalar=par[:, PSC, hd:hd + 1], in1=sa[:],
                        op0=ALU.mult, op1=ALU.mult)))(hd, kya, sa), [psB[kya], B_sa, B_par], [WB["u1"]])
                    for c in range(NTT):
                        def afn(e, c=c):
                            b0 = c * 128
                            e.matmul(psyb(0)[:, b0 + 64:b0 + 128], lhsT=ktb[:, b0:b0 + 128], rhs=qtb[:, b0 + 64:b0 + 128],
                                     start=True, stop=True)
                            return e.matmul(psyb(0)[0:64, b0:b0 + 64], lhsT=ktb[:, b0:b0 + 64], rhs=qtb[:, b0:b0 + 64],
                                            start=True, stop=True)
                        P.op("tensor", afn, [B_ktb, B_qtb], [psyB[0]])
                        P.op("vector", (lambda c: (lambda e: e.tensor_tensor(
                            out=attm[c][:, 64:128], in0=psyb(0)[:, c * 128 + 64:c * 128 + 128], in1=utm[:, 64:128],
                            op=ALU.mult)))(c), [psyB[0], B_const], [B_attm[c]])
                        P.op("vector", (lambda c: (lambda e: e.tensor_tensor(
                            out=attm[c][0:64, 0:64], in0=psyb(0)[0:64, c * 128:c * 128 + 64], in1=utm[0:64, 0:64],
                            op=ALU.mult)))(c), [psyB[0], B_const], [B_attm[c]])
                    for c in range(NTT):
                        cs_ = slice(c * 128, (c + 1) * 128)
                        P.op("tensor", (lambda c, cs_: (lambda e: e.matmul(psyb(2)[:, cs_], lhsT=tok[:, 4 + c, :],
                                                                           rhs=tok[:, c, :], start=True, stop=True)))(c, cs_),
                             [B_tok], [psyB[2]])
                        P.op("vector", (lambda c, cs_: (lambda e: e.tensor_scalar(
                            out=tmpKI[c][:], in0=psyb(2)[:, cs_], scalar1=esc[:, 8 + c:9 + c], scalar2=None,
                            op0=ALU.mult)))(c, cs_), [psyB[2], B_esc], [B_tmpKI[c]])
                    for c in range(NTT):
                        cs_ = slice(c * 128, (c + 1) * 128)
                        P.op("vector", (lambda c, hd: (lambda e: e.tensor_scalar(
                            out=Sp[c % 2][:], in0=Sst[:, hd, :], scalar1=esc[:, c:c + 1], scalar2=None, op0=ALU.mult)))(c, hd),
                            [B_S[hd], B_esc], [B_Sp[c % 2]])

                        def ofn(e, c=c, cs_=cs_):
                            e.matmul(psyb(1)[:, cs_], lhsT=tok[:, c, :], rhs=attm[c][:], start=True, stop=False)
                            return e.matmul(psyb(1)[:, cs_], lhsT=Sp[c % 2][:], rhs=qtb[:, cs_], start=False, stop=True)
                        P.op("tensor", ofn, [B_tok, B_attm[c], B_Sp[c % 2], B_qtb], [psyB[1]])
                        if c < 3 and hd + 1 < 16:
                            early(hd + 1, c)
                        if not (blk == NBLK - 1 and c == NTT - 1):
                            P.op("vector", (lambda c, hd: (lambda e: e.scalar_tensor_tensor(
                                out=Sst[:, hd, :], in0=Sst[:, hd, :], scalar=esc[:, 4 + c:5 + c], in1=tmpKI[c][:],
                                op0=ALU.mult, op1=ALU.add)))(c, hd), [B_S[hd], B_esc, B_tmpKI[c]], [B_S[hd]])
                    P.op("scalar", lambda e: e.activation(out=osqb[:], in_=psyb(1), func=AF.Square), [psyB[1]], [B_osqb])
                    P.op("tensor", lambda e: e.matmul(psyb(3), lhsT=onesb[:], rhs=osqb[:], start=True, stop=True),
                         [B_osqb, B_const], [psyB[3]])
                    P.op("scalar", lambda e: e.activation(out=W["sd"][:], in_=psyb(3), func=AF.Ln, scale=1.0 / 128,
                                                          bias=epsc[:, 0:1]), [psyB[3], B_const], [WB["sd"]])
                    P.op("scalar", lambda e: e.activation(out=W["rstd"][:], in_=W["sd"][:], func=AF.Exp, scale=-0.5),
                         [WB["sd"]], [WB["rstd"]])
                    P.op("vector", lambda e: e.tensor_tensor(out=W["t1"][:], in0=psyb(1), in1=W["rstd"][:], op=ALU.mult),
                         [psyB[1], WB["rstd"]], [WB["t1"]])
                    P.op("vector", (lambda hd, sg: (lambda e: e.scalar_tensor_tensor(
                        out=W["t2"][:], in0=W["t1"][:], scalar=par[:, HGN, hd:hd + 1], in1=sg[:],
                        op0=ALU.mult, op1=ALU.mult)))(hd, sg), [WB["t1"], B_sg, B_par], [WB["t2"]])
                    P.op("gpsimd", (lambda sbg: (lambda e: e.tensor_tensor(out=W["t3"][:], in0=W["t2"][:], in1=sbg[:],
                                                                           op=ALU.mult)))(sbg),
                         [WB["t2"], B_sbg], [WB["t3"]])
                    P.op("gpsimd", (lambda hd: (lambda e: e.tensor_tensor(out=mT[:, hd, :], in0=W["u1"][:], in1=W["t3"][:],
                                                                          op=ALU.add)))(hd), [WB["u1"], WB["t3"]], [B_mT])
                for nch in range(16):
                    wb, wB = wstream("wout", nch)
                    k = pctr[0] % 3
                    pctr[0] += 1

                    def wfn(e, wb=wb, k=k):
                        ins = None
                        for tt in range(NTT):
                            for fc in range(16):
                                ins = e.matmul(ps[k][:, tt * 128:(tt + 1) * 128], lhsT=mT[:, fc, tt * 128:(tt + 1) * 128],
                                               rhs=wb[:, fc, :], start=(fc == 0), stop=(fc == 15))
                        return ins
                    P.op("tensor", wfn, [wB, B_mT], [psB[k]])
                    P.op("vector", (lambda k, nch: (lambda e: e.tensor_tensor(
                        out=o2t[:], in0=ps[k][:, :].rearrange("p (a b) -> p a b", a=NTT),
                        in1=g1bc[:, nch * 128:(nch + 1) * 128].unsqueeze(1).to_broadcast([128, NTT, 128]),
                        op=ALU.mult)))(k, nch), [psB[k], B_g1bc], [B_o2t])
                    P.op("gpsimd", (lambda nch: (lambda e: e.tensor_tensor(
                        out=xt[:, :, nch * 128:(nch + 1) * 128], in0=xt[:, :, nch * 128:(nch + 1) * 128], in1=o2t[:],
                        op=ALU.add)))(nch), [B_o2t] + B_xt, B_xt)
                for tt in range(NTT):
                    P.dma((lambda tt, t0: (lambda e: e.dma_start(out=x1s_d[t0 + tt * 128:t0 + (tt + 1) * 128, :],
                                                                 in_=xt[:, tt, :])))(tt, t0), "d_x1s", reads=[B_xt[tt]],
                          writes=[B_x1s])
                P.wait_all("sync", [B_x1s])
                P.fence()
        if debug == "p1":
            P.emit("main")
            return nc

        THR = 1.0 - 2.0e-6
        with ExitStack() as s2:
            sb = mk_alloc(s2)
            hT = sb("h2T", [128, 16, TB], BF16)
            qM = sb("qM", [128, 16 * TB], BF16)
            qT = qM[:].rearrange("p (g t) -> p g t", g=16)
            Mb1 = sb("Mb1", [128, 16 * TB], BF16)
            Mv = [qM[:].rearrange("p (tt h a b) -> p tt h a b", tt=NTT, h=8, a=2),
                  Mb1[:].rearrange("p (tt h a b) -> p tt h a b", tt=NTT, h=8, a=2)]
            E1b = sb("E1b", [128, NTT, 8, 128], BF16)
            E0p = sb("E0p", [128, NTT, 8, 128])
            Dm = sb("Dm", [128, NTT, 8, 128], BF16)
            yacc = sb("yacc", [128, NTT, D])
            with ExitStack() as sA:
                sba = mk_alloc(sA)
                NWQ = 4
                a_wbf = [sba(f"wbf{i}", [128, 16, 128], BF16) for i in range(NWQ)]
                a_xtmp = [sba(f"xtmp{i}", [128, D]) for i in range(2)]
                a_xn = [sba(f"xn{i}", [128, D]) for i in range(2)]
                a_stat = [sba(f"stat{i}", [128, 4]) for i in range(2)]
                a_Ef = sba("Ef", [128, 16, 128])
                a_Ebt = sba("Ebt", [128, 16, 128], BF16)
                a_wk = sba("wk", [128, 16, 128])
                a_top = sba("top", [128, 16, 16])
                a_cand = sba("cand", [128, 8, 256])
                a_ctop = sba("ctop", [128, 8, 16])
                a_smax = sba("smax", [128, 16])
                a_zz = sba("zz", [128, 32])
                a_kst = sba("kst", [128, 16, 128])
                a_keysb = sba("keysb", [128, 16, 128], BF16)
            with ExitStack() as sM:
                sbm = mk_alloc(sM)
                m_utb = [sbm(f"utb{i}", [128, 16, 128], BF16) for i in range(6)]
                m_vb = [sbm(f"vb{i}", [128, D], BF16) for i in range(8)]
                m_ge = [sbm(f"ge{i}", [128, TB], BF16) for i in range(2)]
                m_AT = [sbm(f"AT{i}", [128, TB], BF16) for i in range(8)]
                m_Pf = [sbm(f"Pf{i}", [128, 8, 2, 128]) for i in range(3)]
            with ExitStack() as sE:
                sbe = mk_alloc(sE)
                e_xtmp = [sbe(f"xe{i}", [128, D]) for i in range(4)]
                e_g2bc = sbe("g2bc", [128, D])
                e_fnbc = sbe("fnbc", [128, D])
                e_junk = [sbe(f"junk{i}", [128, D]) for i in range(4)]
                e_stat = [sbe(f"state{i}", [128, 4]) for i in range(4)]

            def peer_prologue(blk):
                t0 = blk * TB
                wbf, xtmp, xn, stat, Ef, Ebt, wk, top, cand, ctop, smax, zz, kst, keysb = (
                    a_wbf, a_xtmp, a_xn, a_stat, a_Ef, a_Ebt, a_wk, a_top, a_cand, a_ctop, a_smax, a_zz, a_kst,
                    a_keysb)
                B_wbf = P.bufs(NWQ, "wbf2")
                B_xtmp = P.bufs(2, "xtmp")
                B_xn, B_stat, B_Ef, B_Ebt, B_wk, B_top, B_cand, B_ctop, B_smax, B_zz, B_kst, B_keys = (
                    P.buf(n) for n in ("xn2", "stat2", "Ef", "Ebt", "wk", "top", "cand", "ctop", "smax", "zz", "kst",
                                       "keysb"))
                B_hT = P.buf("h2T")
                B_qT = P.buf("qT")
                B_E1 = P.bufs(NTT, "E1b")
                B_E0 = P.bufs(NTT, "E0p")
                B_Dm = P.bufs(NTT, "Dm")
                P.dma(lambda e: e.dma_start(out=kst[:], in_=keysT_d), "d_kst", writes=[B_kst])
                P.op("vector", lambda e: e.tensor_copy(out=keysb[:], in_=kst[:]), [B_kst], [B_keys])
                pctr = [0]
                B_xn2 = [B_xn, P.buf("xn2b")]
                B_stat2 = [B_stat, P.buf("stat2b")]

                def norm_tt(tt, xn, stat, B_xn, B_stat):
                        P.dma((lambda tt: (lambda e: e.dma_start(out=xtmp[tt % 2][:],
                                                                 in_=x1s_d[t0 + tt * 128:t0 + (tt + 1) * 128, :])))(tt),
                              f"d_xtmp{tt % 2}", writes=[B_xtmp[tt % 2]])
                        src = xtmp[tt % 2][:]
                        B_src = B_xtmp[tt % 2]
                        P.op("scalar", (lambda src: (lambda e: e.activation(out=xn[:], in_=src, func=AF.Square)))(src),
                             [B_src], [B_xn])
                        P.op("vector", lambda e: e.tensor_reduce(out=stat[:, 0:1], in_=xn[:], axis=AX.X, op=ALU.add),
                             [B_xn], [B_stat])
                        P.op("scalar", lambda e: e.activation(out=stat[:, 1:2], in_=stat[:, 0:1], func=AF.Ln,
                                                              scale=1.0 / D, bias=epsc[:, 0:1]), [B_stat, B_const], [B_stat])
                        P.op("scalar", lambda e: e.activation(out=stat[:, 2:3], in_=stat[:, 1:2], func=AF.Exp, scale=-0.5),
                             [B_stat], [B_stat])
                        P.op("vector", (lambda src: (lambda e: e.tensor_scalar(out=xn[:], in0=src, scalar1=stat[:, 2:3],
                                                                               scalar2=None, op0=ALU.mult)))(src),
                             [B_src, B_stat], [B_xn])
                        for gq in range(4):
                            k = pctr[0] % 3
                            pctr[0] += 1

                            def tfn(e, gq=gq, k=k):
                                ins = None
                                for j in range(4):
                                    dc = gq * 4 + j
                                    ins = e.transpose(out=ps[k][:, j * 128:(j + 1) * 128],
                                                      in_=xn[:, dc * 128:(dc + 1) * 128], identity=identf[:])
                                return ins
                            P.op("tensor", tfn, [B_xn, B_const], [psB[k]])
                            for j in range(4):
                                dc = gq * 4 + j
                                o_ap = hT[:, dc, tt * 128:(tt + 1) * 128]
                                i_ap = ps[k][:, j * 128:(j + 1) * 128]
                                if j % 2 == 0:
                                    P.op("scalar", (lambda o_ap, i_ap, dc: (lambda e: e.activation(
                                        out=o_ap, in_=i_ap, func=AF.Identity, scale=par[:, A2, dc:dc + 1],
                                        bias=par[:, SH2, dc:dc + 1])))(o_ap, i_ap, dc), [psB[k], B_par], [B_hT])
                                else:
                                    P.op("vector", (lambda o_ap, i_ap, dc: (lambda e: e.tensor_scalar(
                                        out=o_ap, in0=i_ap, scalar1=par[:, A2, dc:dc + 1], scalar2=par[:, SH2, dc:dc + 1],
                                        op0=ALU.mult, op1=ALU.add)))(o_ap, i_ap, dc), [psB[k], B_par], [B_hT])

                for tt in range(NTT):
                    norm_tt(tt, xn[tt % 2], stat[tt % 2], B_xn2[tt % 2], B_stat2[tt % 2])
                for ch in range(16):
                    s = ch % NWQ
                    if ch == 0:
                        for c2 in range(min(3, 16)):
                            P.dma((lambda c2: (lambda e: e.dma_start(out=wbf[c2 % NWQ][:], in_=wq_d[c2])))(c2),
                                  f"d_wbfq{c2 % NWQ}", writes=[B_wbf[c2 % NWQ]], eng="gpsimd")
                    if ch + 3 < 16:
                        c2 = ch + 3
                        P.dma((lambda c2: (lambda e: e.dma_start(out=wbf[c2 % NWQ][:], in_=wq_d[c2])))(c2),
                              f"d_wbfq{c2 % NWQ}", writes=[B_wbf[c2 % NWQ]], eng="gpsimd")
                    k = pctr[0] % 3
                    pctr[0] += 1
                    mm_group(P, ps[k][:, :], [(wbf[s][:, kc, :], hT[:, kc, :]) for kc in range(16)],
                             [B_wbf[s], B_hT], [psB[k]])
                    P.op("scalar", (lambda ch, k: (lambda e: e.copy(out=qT[:, ch, :], in_=ps[k][:, :])))(ch, k),
                         [psB[k]], [B_qT])
                for tt in range(NTT):
                    for bk in range(4):
                        def sfn(e, tt=tt, bk=bk):
                            ins = None
                            for j in range(4):
                                g = bk * 4 + j
                                ins = e.matmul(psyb(bk)[:, j * 128:(j + 1) * 128], lhsT=qT[:, g, tt * 128:(tt + 1) * 128],
                                               rhs=keysb[:, g, :], start=True, stop=True)
                            return ins
                        P.op("tensor", sfn, [B_qT, B_keys], [psyB[bk]])
                        pv3 = psyb(bk).rearrange("p (a b) -> p a b", a=4)
                        P.op("vector", (lambda bk, pv3: (lambda e: e.tensor_reduce(
                            out=smax[:, bk * 4:bk * 4 + 4], in_=pv3, axis=AX.X, op=ALU.max)))(bk, pv3),
                            [psyB[bk]], [B_smax])
                        P.op("vector", (lambda bk, pv3: (lambda e: e.tensor_tensor(
                            out=Ef[:, bk * 4:bk * 4 + 4, :], in0=pv3,
                            in1=smax[:, bk * 4:bk * 4 + 4].unsqueeze(2).to_broadcast([128, 4, 128]),
                            op=ALU.subtract)))(bk, pv3), [psyB[bk], B_smax], [B_Ef])
                    P.op("scalar", lambda e: e.activation(out=Ebt[:], in_=Ef[:], func=AF.Exp), [B_Ef], [B_Ebt])
                    P.op("vector", lambda e: e.tensor_copy(out=Ef[:], in_=Ebt[:]), [B_Ebt], [B_Ef])
                    Eb4 = Ebt[:].rearrange("p (h two) n -> p h two n", two=2)
                    Ef4 = Ef[:].rearrange("p (h two) n -> p h two n", two=2)
                    P.op("gpsimd", (lambda tt, Eb4: (lambda e: e.tensor_copy(out=E1b[:, tt, :, :], in_=Eb4[:, :, 1, :])))(tt, Eb4),
                         [B_Ebt], [B_E1[tt]])
                    B_topg = P.bufs(16, "topg")
                    B_wkg = P.bufs(16, "wkg")
                    for g in range(16):
                        P.op("vector", (lambda g: (lambda e: e.max(out=top[:, g, 0:8], in_=Ef[:, g, :])))(g),
                             [B_Ef, B_top], [B_topg[g]])
                    for g in range(16):
                        P.op("vector", (lambda g: (lambda e: e.match_replace(
                            out=wk[:, g, :], in_to_replace=top[:, g, 0:8], in_values=Ef[:, g, :], imm_value=NEG)))(g),
                            [B_Ef, B_topg[g], B_wk], [B_wkg[g]])
                    for g in range(16):
                        P.op("vector", (lambda g: (lambda e: e.max(out=top[:, g, 8:16], in_=wk[:, g, :])))(g),
                             [B_wkg[g]], [B_topg[g]])
                    top4 = top[:].rearrange("p (h two) k -> p h two k", two=2)
                    P.op("vector", lambda e: e.tensor_tensor(
                        out=cand[:].rearrange("p h (i j) -> p h i j", i=16),
                        in0=top4[:, :, 0, :].unsqueeze(3).to_broadcast([128, 8, 16, 16]),
                        in1=top4[:, :, 1, :].unsqueeze(2).to_broadcast([128, 8, 16, 16]), op=ALU.mult),
                        B_topg, [B_cand, B_top])
                    wk8 = wk[:].rearrange("p (h a) b -> p h (a b)", a=2)
                    B_ctoph = P.bufs(8, "ctoph")
                    B_wkh = P.bufs(8, "wkh")
                    for h in range(8):
                        P.op("vector", (lambda h: (lambda e: e.max(out=ctop[:, h, 0:8], in_=cand[:, h, :])))(h),
                             [B_cand, B_ctop], [B_ctoph[h]])
                    for h in range(8):
                        P.op("vector", (lambda h, wk8: (lambda e: e.match_replace(
                            out=wk8[:, h, :], in_to_replace=ctop[:, h, 0:8], in_values=cand[:, h, :], imm_value=NEG)))(h, wk8),
                            [B_cand, B_ctoph[h]] + B_wkg, [B_wkh[h]])
                    for h in range(8):
                        P.op("vector", (lambda h, wk8: (lambda e: e.max(out=ctop[:, h, 8:16], in_=wk8[:, h, :])))(h, wk8),
                             [B_wkh[h]], [B_ctoph[h]])
                    B_ctop_all = B_ctoph
                    P.op("vector", lambda e: e.reciprocal(out=zz[:, 24:32], in_=ctop[:, :, 15]), B_ctop_all, [B_zz, B_ctop, B_wk])
                    P.op("vector", (lambda tt, Ef4: (lambda e: e.tensor_tensor(
                        out=E0p[:, tt, :, :], in0=Ef4[:, :, 0, :],
                        in1=zz[:, 24:32].unsqueeze(2).to_broadcast([128, 8, 128]), op=ALU.mult)))(tt, Ef4),
                        [B_Ef, B_zz], [B_E0[tt]])
                    P.op("vector", lambda e: e.tensor_reduce(out=zz[:, 0:8], in_=ctop[:], axis=AX.X, op=ALU.add),
                         B_ctop_all, [B_zz])
                    P.op("vector", lambda e: e.reciprocal(out=zz[:, 8:16], in_=zz[:, 0:8]), [B_zz], [B_zz])
                    P.op("vector", lambda e: e.tensor_tensor(out=zz[:, 16:24], in0=zz[:, 8:16], in1=ctop[:, :, 15],
                                                             op=ALU.mult), [B_zz] + B_ctop_all, [B_zz])
                    for h in range(8):
                        P.op("gpsimd", (lambda tt, h: (lambda e: e.tensor_scalar(
                            out=Dm[:, tt, h, :], in0=identf[:], scalar1=zz[:, 16 + h:17 + h], scalar2=None,
                            op0=ALU.mult)))(tt, h), [B_zz, B_const], [B_Dm[tt]])
                return B_hT, B_E1, B_E0, B_Dm

            def peer_main(blk, B_hT, B_E1, B_E0, B_Dm):
                utb, vb, ge, AT, Pf = m_utb, m_vb, m_ge, m_AT, m_Pf
                B_utb = P.bufs(6, "utb")
                B_vb = P.bufs(8, "vb")
                B_ge = P.bufs(2, "ge")
                B_AT = P.bufs(8, "AT")
                B_Pf = P.bufs(3, "Pf")
                B_M = [P.bufs(NTT, "M0_"), P.bufs(NTT, "M1_")]
                B_yacc = P.bufs(NTT, "yacc")
                pfc = [0]

                def stageA(sc):
                    mv = Mv[sc % 2]
                    for tt in range(NTT):
                        pf = Pf[pfc[0] % 3]
                        B_pf = B_Pf[pfc[0] % 3]
                        pfc[0] += 1
                        if tt % 2 == 0:
                            P.op("gpsimd", (lambda pf, tt, sc: (lambda e: e.tensor_tensor(
                                out=pf[:],
                                in0=E0p[:, tt, :, 2 * sc:2 * sc + 2].unsqueeze(3).to_broadcast([128, 8, 2, 128]),
                                in1=E1b[:, tt, :, :].unsqueeze(2).to_broadcast([128, 8, 2, 128]), op=ALU.mult)))(pf, tt, sc),
                                [B_E0[tt], B_E1[tt]], [B_pf])
                        else:
                            def pfn(e, pf=pf, tt=tt, sc=sc):
                                ins = None
                                for h in range(8):
                                    for a in range(2):
                                        ins = e.activation(out=pf[:, h, a, :], in_=E1b[:, tt, h, :], func=AF.Copy,
                                                           scale=E0p[:, tt, h, 2 * sc + a:2 * sc + a + 1])
                                return ins
                            P.op("scalar", pfn, [B_E0[tt], B_E1[tt]], [B_pf])
                        P.op("vector", (lambda pf, tt, mv: (lambda e: e.scalar_tensor_tensor(
                            out=mv[:, tt, :, :, :], in0=pf[:], scalar=THR, in1=pf[:],
                            op0=ALU.is_ge, op1=ALU.mult)))(pf, tt, mv), [B_pf], [B_M[sc % 2][tt]])

                def stageDU(sc):
                    for k in range(2):
                        et = 2 * sc + k
                        P.dma((lambda et: (lambda e: e.dma_start(out=utb[et % 6][:], in_=UT_d[et])))(et),
                              f"d_utb{et % 6}", writes=[B_utb[et % 6]], eng="gpsimd")

                def stageDV(sc):
                    for k in range(2):
                        et = 2 * sc + k
                        P.dma((lambda et: (lambda e: e.dma_start(out=vb[et % 8][:], in_=V_d[et])))(et),
                              f"d_vb{et % 8}", writes=[B_vb[et % 8]], eng="gpsimd")

                def stageB(sc):
                    mv = Mv[sc % 2]
                    for k in range(2):
                        et = 2 * sc + k
                        s4 = et % 8
                        u6 = et % 6
                        mm_group(P, ps[k][:, :], [(utb[u6][:, dc, :], hT[:, dc, :]) for dc in range(16)],
                                 [B_utb[u6], B_hT], [psB[k]])

                        def gfn(e, k=k, mv=mv):
                            ins = None
                            for tt in range(NTT):
                                for h in range(8):
                                    ins = e.matmul(ps[2 + k][:, tt * 128:(tt + 1) * 128], lhsT=mv[:, tt, h, k, :],
                                                   rhs=Dm[:, tt, h, :], start=(h == 0), stop=(h == 7))
                            return ins
                        P.op("tensor", gfn, B_M[sc % 2] + B_Dm, [psB[2 + k]])
                        P.op("scalar", (lambda k: (lambda e: e.activation(out=ge[k][:], in_=ps[k][:, :], func=AF.Gelu)))(k),
                             [psB[k]], [B_ge[k]])
                        P.op("vector", (lambda k, s4: (lambda e: e.tensor_tensor(out=AT[s4][:], in0=ps[2 + k][:, :],
                                                                                 in1=ge[k][:], op=ALU.mult)))(k, s4),
                             [psB[2 + k], B_ge[k]], [B_AT[s4]])

                def stageC(j):
                    et0 = 4 * j
                    sl = [(et0 + k) % 8 for k in range(4)]
                    for tt in range(NTT):
                        for hf in range(2):
                            def yfn(e, tt=tt, hf=hf, sl=sl):
                                ins = None
                                for n in (2 * hf, 2 * hf + 1):
                                    for k in range(4):
                                        ins = e.matmul(psyb(n), lhsT=AT[sl[k]][:, tt * 128:(tt + 1) * 128],
                                                       rhs=vb[sl[k]][:, n * 512:(n + 1) * 512], start=(k == 0),
                                                       stop=(k == 3))
                                return ins
                            pb = [psyB[2 * hf], psyB[2 * hf + 1]]
                            P.op("tensor", yfn, [B_AT[i] for i in sl] + [B_vb[i] for i in sl], pb)
                            cs_ = slice(hf * 1024, (hf + 1) * 1024)
                            if j == 0:
                                P.op("vector", (lambda tt, cs_: (lambda e: e.tensor_copy(out=yacc[:, tt, cs_],
                                                                                         in_=psy[:, cs_])))(tt, cs_),
                                     pb, [B_yacc[tt]])
                            else:
                                P.op("vector", (lambda tt, cs_: (lambda e: e.tensor_tensor(
                                    out=yacc[:, tt, cs_], in0=psy[:, cs_], in1=yacc[:, tt, cs_], op=ALU.add)))(tt, cs_),
                                    pb + [B_yacc[tt]], [B_yacc[tt]])

                NSC = 64
                stageDU(0)
                stageDU(1)
                stageDV(0)
                stageA(0)
                for sc in range(NSC):
                    if sc + 1 < NSC:
                        stageA(sc + 1)
                    stageB(sc)
                    if sc + 2 < NSC:
                        stageDU(sc + 2)
                    if sc + 1 < NSC:
                        stageDV(sc + 1)
                    if sc % 2 == 0 and sc >= 2:
                        stageC((sc - 2) // 2)
                stageC(NSC // 2 - 1)
                return B_yacc

            def peer_epilogue(blk, B_yacc):
                t0 = blk * TB
                xtmp, g2bc, fnbc = e_xtmp, e_g2bc, e_fnbc
                B_xe = P.bufs(4, "xe")
                B_bc = P.buf("bc2")
                B_out = P.buf("out")
                P.dma(lambda e: e.dma_start(out=g2bc[:], in_=g2s_d.partition_broadcast(128)), "d_bc2", writes=[B_bc],
                      eng="scalar")
                P.dma(lambda e: e.dma_start(out=fnbc[:], in_=fnorm_d.partition_broadcast(128)), "d_bc2", writes=[B_bc],
                      eng="scalar")

                def ep_load(tt, s):
                    P.dma(lambda e: e.dma_start(out=xtmp[s][:], in_=x1s_d[t0 + tt * 128:t0 + (tt + 1) * 128, :]),
                          f"d_xe{s}", writes=[B_xe[s]])
                for tt in range(NTT):
                    ep_load(tt, tt)

                B_junk4 = P.bufs(4, "junk")
                B_st4 = P.bufs(4, "state")

                def ep_stage(k, tt):
                    s = tt
                    junk, stat, B_junk, B_st = e_junk[tt], e_stat[tt], B_junk4[tt], B_st4[tt]
                    if k == 0:
                        P.op("gpsimd", lambda e: e.tensor_tensor(out=yacc[:, tt, :], in0=yacc[:, tt, :], in1=g2bc[:],
                                                                 op=ALU.mult), [B_yacc[tt], B_bc], [B_yacc[tt]])
                        P.op("vector", lambda e: e.tensor_tensor(out=xtmp[s][:], in0=xtmp[s][:], in1=yacc[:, tt, :],
                                                                 op=ALU.add), [B_xe[s], B_yacc[tt]], [B_xe[s]])
                        P.op("scalar", lambda e: e.activation(out=junk[:], in_=xtmp[s][:], func=AF.Square),
                             [B_xe[s]], [B_junk])
                    elif k == 1:
                        P.op("vector", lambda e: e.tensor_reduce(out=stat[:, 0:1], in_=junk[:], axis=AX.X, op=ALU.add),
                             [B_junk], [B_st])
                        P.op("scalar", lambda e: e.activation(out=stat[:, 1:2], in_=stat[:, 0:1], func=AF.Ln,
                                                              scale=1.0 / D, bias=epsc[:, 0:1]), [B_st, B_const], [B_st])
                        P.op("scalar", lambda e: e.activation(out=stat[:, 2:3], in_=stat[:, 1:2], func=AF.Exp, scale=-0.5),
                             [B_st], [B_st])
                    else:
                        P.op("vector", lambda e: e.scalar_tensor_tensor(
                            out=xtmp[s][:], in0=xtmp[s][:], scalar=stat[:, 2:3], in1=fnbc[:], op0=ALU.mult,
                            op1=ALU.mult), [B_xe[s], B_st, B_bc], [B_xe[s]])
                        P.dma(lambda e: e.dma_start(out=out_d[t0 + tt * 128:t0 + (tt + 1) * 128, :], in_=xtmp[s][:]),
                              f"d_out{s}", reads=[B_xe[s]], writes=[B_out])
                for k in range(3):
                    for tt in range(NTT):
                        ep_stage(k, tt)
                return B_out

            outs = []
            for blk in range(NBLK):
                P.fence()
                r = peer_prologue(blk)
                P.fence()
                if debug == "p2a":
                    dbg2 = nc.dram_tensor("dbg2", [128, NTT * 8 * 128 * 3], F32, kind="ExternalOutput").ap()
                    cv = sb("cv", [128, NTT * 8 * 128])
                    B_cv = P.buf("cv")
                    B_o = P.buf("dbgo")
                    n1 = NTT * 8 * 128
                    P.dma(lambda e: e.dma_start(out=dbg2[:, 0:n1], in_=E0p[:].rearrange("p a b c -> p (a b c)")), "d_dbg2",
                          reads=r[2], writes=[B_o])
                    P.op("vector", lambda e: e.tensor_copy(out=cv[:], in_=E1b[:].rearrange("p a b c -> p (a b c)")),
                         r[1], [B_cv])
                    P.dma(lambda e: e.dma_start(out=dbg2[:, n1:2 * n1], in_=cv[:]), "d_dbg2", reads=[B_cv], writes=[B_o])
                    cv2 = sb("cv2", [128, NTT * 8 * 128])
                    B_cv2 = P.buf("cv2")
                    P.op("vector", lambda e: e.tensor_copy(out=cv2[:], in_=Dm[:].rearrange("p a b c -> p (a b c)")),
                         r[3], [B_cv2])
                    P.dma(lambda e: e.dma_start(out=dbg2[:, 2 * n1:3 * n1], in_=cv2[:]), "d_dbg2", reads=[B_cv2], writes=[B_o])
                    P.wait_all("sync", [B_o])
                    P.emit("main")
                    return nc
                B_yacc = peer_main(blk, *r)
                P.fence()
                if debug == "p2m":
                    dbg3 = nc.dram_tensor("dbg3", [128, NTT * D], F32, kind="ExternalOutput").ap()
                    B_o = P.buf("dbgo")
                    P.dma(lambda e: e.dma_start(out=dbg3, in_=yacc[:].rearrange("p a b -> p (a b)")), "d_dbg3",
                          reads=B_yacc, writes=[B_o])
                    P.wait_all("sync", [B_o])
                    P.emit("main")
                    return nc
                outs.append(peer_epilogue(blk, B_yacc))
                if debug == "p2e" and blk == 1:
                    break
            P.wait_all("sync", outs)
        P.emit("main")
    return nc


def _tile_w(w):
    K, N = w.shape
    return np.ascontiguousarray(w.reshape(16, 128, N // 128, 128).transpose(2, 1, 0, 3))


def _cols(v):
    return np.ascontiguousarray(np.asarray(v, np.float32).reshape(-1, 128).T)


def prep_inputs(x, c, w_ada, b_ada, norm1, w_in, pool_w, pool_scale, lb_logits, hg_norm, w_out, norm2, peer_wq,
                peer_keys, peer_u, peer_v, final_norm):
    f = lambda a: np.asarray(a, np.float32)
    sh = {}
    wa = f(w_ada)[0]
    sh["wada"] = np.ascontiguousarray(wa.reshape(16, 128, 24, 512).transpose(2, 1, 0, 3))
    sh["bada"] = _cols(f(b_ada)[0])
    sh["pvec"] = np.ascontiguousarray(np.stack([_cols(f(norm1)[0]), _cols(f(norm2)[0]), _cols(f(pool_scale)[0]),
                                                _cols(f(hg_norm)[0].reshape(-1)), _cols(f(lb_logits)[0]),
                                                _cols(f(lb_logits)[1])], axis=1))
    sh["fnorm"] = np.ascontiguousarray(f(final_norm))
    sh["win"] = _tile_w(f(w_in)[0])
    sh["poolw"] = np.ascontiguousarray(f(pool_w)[0].reshape(4, 2, 128, 512).transpose(2, 0, 1, 3).reshape(128, 8, 512))
    sh["wout"] = _tile_w(f(w_out)[0])
    sh["wq"] = _tile_w(f(peer_wq)[0])
    sh["keysT"] = np.ascontiguousarray(f(peer_keys)[0].transpose(3, 0, 1, 2).reshape(128, 16, 128))
    sh["UT"] = np.ascontiguousarray(f(peer_u)[0].reshape(128, 128, 16, 128).transpose(0, 3, 2, 1))
    sh["V"] = np.ascontiguousarray(f(peer_v)[0].reshape(128, 128, D))
    sh["ident"] = np.eye(128, dtype=np.float32)
    sh["utm"] = np.triu(np.ones((128, 128), np.float32))
    pinv = np.zeros((128, 4, 16), np.float32)
    for g, w in enumerate((2, 4, 8, 16)):
        pinv[:, g, :] = 1.0 / np.minimum(np.arange(16) + 1, w).astype(np.float32)[None, :]
    sh["pinv"] = pinv
    xs = f(x)
    cs = f(c)
    maps = []
    for b in range(xs.shape[0]):
        m = dict(sh)
        m["x"] = np.ascontiguousarray(xs[b])
        m["cT"] = _cols(cs[b])
        maps.append(m)
    return maps


def kernel(**inputs):
    maps = prep_inputs(**inputs)
    nc = build_program()
    res = run_bass_kernel_spmd(nc, maps, core_ids=list(range(len(maps))))
    out = np.stack([np.asarray(r["out"], np.float32) for r in res.results], axis=0)
    return out
```

```python
from contextlib import ExitStack
import numpy as np
import ml_dtypes
import concourse.bass as bass
import concourse.mybir as mybir
from concourse.bass_utils import run_bass_kernel_spmd

F32 = mybir.dt.float32
BF16 = mybir.dt.bfloat16
AF = mybir.ActivationFunctionType
ALU = mybir.AluOpType
AX = mybir.AxisListType

D = 2048
SEQ = 2048
TB = 512
NBLK = SEQ // TB
NTT = TB // 128
EPS = 1e-6
NEG = -1.0e30
SAME_ENGINE_RAW = True


class Buf:
    __slots__ = ("name", "w", "r")

    def __init__(self, name):
        self.name = name
        self.w = {}
        self.r = {}


class Prog:
    ENGS = ("sync", "scalar", "vector", "gpsimd", "tensor")

    def __init__(self, nc, stack):
        self.nc = nc
        self.stack = stack
        self.sems = {}
        self.cnt = {}
        self.known = {e: {} for e in self.ENGS}
        self.ops = []
        self.nbuf = 0
        self.base = {}
        for e in self.ENGS[1:]:
            self._sem("E_" + e)

    def _sem(self, key):
        if key not in self.sems:
            self.sems[key] = self.stack.enter_context(self.nc.semaphore(key))
            self.cnt[key] = 0
        return self.sems[key]

    def buf(self, name=None):
        self.nbuf += 1
        b = Buf(name or f"b{self.nbuf}")
        b.w = dict(self.base)
        return b

    def fence(self):
        self.base = {k: v for k, v in self.cnt.items() if v > 0}

    def bufs(self, n, name="b"):
        return [self.buf(f"{name}{i}") for i in range(n)]

    def _collect(self, eng, reads, writes):
        need = {}
        own = "E_" + eng

        def add(d, raw):
            for k, v in d.items():
                if k == own and (eng == "tensor" or not SAME_ENGINE_RAW):
                    continue
                if need.get(k, 0) < v:
                    need[k] = v

        for b in reads:
            add(b.w, True)
        for b in writes:
            add(b.w, False)
            add(b.r, False)
        kn = self.known[eng]
        waits = []
        for k, v in need.items():
            if kn.get(k, 0) >= v:
                continue
            kn[k] = v
            waits.append((k, v))
        return waits

    def op(self, eng, fn, reads=(), writes=()):
        waits = self._collect(eng, reads, writes)
        key = "E_" + eng
        self.cnt[key] += 1
        v = self.cnt[key]
        for b in reads:
            if b.r.get(key, 0) < v:
                b.r[key] = v
        for b in writes:
            if b.w.get(key, 0) < v:
                b.w[key] = v
        self.ops.append((eng, fn, waits, (key, 1)))

    def dma(self, fn, semkey, reads=(), writes=(), eng="sync"):
        self._sem(semkey)
        waits = self._collect(eng, reads, writes)
        self.cnt[semkey] += 16
        v = self.cnt[semkey]
        for b in reads:
            if b.r.get(semkey, 0) < v:
                b.r[semkey] = v
        for b in writes:
            if b.w.get(semkey, 0) < v:
                b.w[semkey] = v
        self.ops.append((eng, fn, waits, (semkey, 16)))

    def wait_all(self, eng, bufs, also_reads=False):
        need = []
        if also_reads:
            waits = self._collect(eng, (), bufs)
        else:
            waits = self._collect(eng, bufs, ())
        self.ops.append((eng, None, waits, None))

    def emit(self, name=None):
        nc = self.nc
        ops = self.ops
        sems = self.sems
        import bisect
        waited = {}
        for (_, _, waits, _) in ops:
            for (k, v) in waits:
                if k.startswith("E_"):
                    waited.setdefault(k, set()).add(v)
        waited = {k: sorted(vs) for k, vs in waited.items()}
        base = getattr(self, "_rank_base", {})

        def rank(k, v):
            return base.get(k, 0) + bisect.bisect_right(waited.get(k, []), v)

        seqs = getattr(self, "_seq_base", {})
        plan = []
        cur = dict(seqs)
        for (eng, fn, waits, inc) in ops:
            w2 = []
            for (k, v) in waits:
                if k.startswith("E_"):
                    w2.append((k, rank(k, v)))
                else:
                    w2.append((k, v))
            sig = None
            if inc is not None:
                k, amt = inc
                if k.startswith("E_"):
                    cur[k] = cur.get(k, 0) + 1
                    n = cur[k]
                    lst = waited.get(k, [])
                    i = bisect.bisect_left(lst, n)
                    if i < len(lst) and lst[i] == n:
                        sig = (k, 1)
                else:
                    sig = (k, amt)
            plan.append((eng, fn, w2, sig))
        with nc.Block(name) as blk:
            for e in self.ENGS:
                mine = [o for o in plan if o[0] == e]
                if not mine:
                    continue

                def body(eng, mine=mine):
                    for (_, fn, waits, sig) in mine:
                        for (k, v) in waits:
                            eng.wait_ge(sems[k], v)
                        if fn is None:
                            continue
                        ins = fn(eng)
                        if sig is not None:
                            ins.then_inc(sems[sig[0]], sig[1])

                getattr(blk, e)(body)
        nsig = sum(1 for o in plan if o[3] is not None)
        self.stats = (len(plan), nsig)
        self.ops = []


def mm_group(P, out_ap, pairs, reads, writes):
    pairs = list(pairs)

    def fn(e):
        n = len(pairs)
        ins = None
        for i, (l, r) in enumerate(pairs):
            ins = e.matmul(out_ap, lhsT=l, rhs=r, start=(i == 0), stop=(i == n - 1))
        return ins

    P.op("tensor", fn, reads, writes)


def build_program(debug=None):
    nc = bass.Bass("TRN2", target_bir_lowering=False)
    dt_in = lambda n, s: nc.dram_tensor(n, s, F32, kind="ExternalInput").ap()
    x_d = dt_in("x", [SEQ, D])
    cT_d = dt_in("cT", [128, 16])
    wada_d = dt_in("wada", [24, 128, 16, 512])
    bada_d = dt_in("bada", [128, 96])
    pvec_d = dt_in("pvec", [128, 6, 16])
    fnorm_d = dt_in("fnorm", [D])
    win_d = dt_in("win", [104, 128, 16, 128])
    poolw_d = dt_in("poolw", [128, 8, 512])
    wout_d = dt_in("wout", [16, 128, 16, 128])
    wq_d = dt_in("wq", [16, 128, 16, 128])
    keysT_d = dt_in("keysT", [128, 16, 128])
    UT_d = dt_in("UT", [128, 128, 16, 128])
    V_d = dt_in("V", [128, 128, D])
    ident_d = dt_in("ident", [128, 128])
    utm_d = dt_in("utm", [128, 128])
    pinv_d = dt_in("pinv", [128, 4, 16])
    out_d = nc.dram_tensor("out", [SEQ, D], F32, kind="ExternalOutput").ap()
    x1_kind = "ExternalOutput" if debug in ("p1", "zero") else "Internal"
    x1s_d = nc.dram_tensor("x1s", [SEQ, D], F32, kind=x1_kind).ap()
    g1s_d = nc.dram_tensor("g1s", [D], F32, kind=x1_kind).ap()
    g2s_d = nc.dram_tensor("g2s", [D], F32, kind=x1_kind).ap()
    dbg_d = None
    if debug == "p0":
        dbg_d = nc.dram_tensor("dbg", [128, 160], F32, kind="ExternalOutput").ap()

    with ExitStack() as gs:
        P = Prog(nc, gs)

        uid = [0]

        def mk_alloc(stack):
            def sb(name, shape, dt=F32):
                uid[0] += 1
                return stack.enter_context(nc.sbuf_tensor(f"{name}_{uid[0]}", shape, dt))
            return sb

        gsb = mk_alloc(gs)
        identf = gsb("identf", [128, 128])
        identb = gsb("identb", [128, 128], BF16)
        utm = gsb("utm_sb", [128, 128])
        onesf = gsb("onesf", [128, 128])
        onesb = gsb("onesb", [128, 128], BF16)
        pinv = gsb("pinv_sb", [128, 4, 16])
        pv = gsb("pv", [128, 6, 16])
        par = gsb("par", [128, 12, 16])
        A1, SH1, G1C, A2, SH2, G2C, PSC, HGN, LB, OML = range(10)
        epsc = gsb("epsc", [128, 1])
        ps = [gs.enter_context(nc.psum_tensor(f"ps{i}", [128, 512], F32)) for i in range(4)]
        psy = gs.enter_context(nc.psum_tensor("psy", [128, 2048], F32))
        psB = [P.buf(f"ps{i}") for i in range(4)]
        psyB = [P.buf(f"psy{i}") for i in range(4)]

        def psyb(i):
            return psy[:, i * 512:(i + 1) * 512]

        B_const = P.buf("consts")
        B_par = P.buf("par")

        with ExitStack() as s0:
            sb = mk_alloc(s0)
            NWA = 4
            wa = [sb(f"wa{i}", [128, 16, 512], BF16) for i in range(NWA)]
            waB = P.bufs(NWA, "wa")
            csb = sb("csb", [128, 16], BF16)
            cst = sb("cst", [128, 16])
            cs = sb("cs", [128, 16])
            bada = sb("bada_sb", [128, 96])
            ada = sb("ada_sb", [128, 96])
            tmpc = sb("tmpc", [128, 16])
            B_c = P.buf("c")
            B_ada = P.buf("ada")
            for (dst, src) in ((identf, ident_d), (utm, utm_d), (pinv, pinv_d), (pv, pvec_d),
                               (cst, cT_d), (bada, bada_d)):
                P.dma((lambda d, s: (lambda e: e.dma_start(out=d[:], in_=s)))(dst, src), "d_const",
                      writes=[B_const])
            P.op("vector", lambda e: e.tensor_copy(out=identb[:], in_=identf[:]), [B_const], [B_const])
            P.op("gpsimd", lambda e: e.memset(onesf[:], 1.0), [], [B_const])
            P.op("gpsimd", lambda e: e.memset(onesb[:], 1.0), [], [B_const])
            P.op("gpsimd", lambda e: e.memset(epsc[:], EPS), [], [B_const])
            P.op("scalar", lambda e: e.activation(out=cs[:], in_=cst[:], func=AF.Silu), [B_const], [B_c])
            P.op("vector", lambda e: e.tensor_copy(out=csb[:], in_=cs[:]), [B_c], [B_c])
            for j in range(24):
                P.dma((lambda j: (lambda e: e.dma_start(out=wa[j % NWA][:], in_=wada_d[j])))(j), f"d_wa{j % NWA}",
                      writes=[waB[j % NWA]], eng="gpsimd")

                def fn(e, j=j):
                    ins = None
                    for q in range(4):
                        col = j * 4 + q
                        for kc in range(16):
                            ins = e.matmul(ps[0][:, col:col + 1], lhsT=wa[j % NWA][:, kc, q * 128:(q + 1) * 128],
                                           rhs=csb[:, kc:kc + 1], start=(kc == 0), stop=(kc == 15))
                    return ins
                P.op("tensor", fn, [waB[j % NWA], B_c], [psB[0]])
            P.op("vector", lambda e: e.tensor_tensor(out=ada[:], in0=ps[0][:, 0:96], in1=bada[:], op=ALU.add),
                 [psB[0], B_const], [B_ada])
            P.op("vector", lambda e: e.scalar_tensor_tensor(out=par[:, A1, :], in0=ada[:, 16:32], scalar=1.0,
                                                            in1=pv[:, 0, :], op0=ALU.add, op1=ALU.mult),
                 [B_ada, B_const], [B_par])
            P.op("vector", lambda e: e.tensor_copy(out=par[:, SH1, :], in_=ada[:, 0:16]), [B_ada], [B_par])
            P.op("vector", lambda e: e.tensor_scalar(out=par[:, G1C, :], in0=ada[:, 32:48], scalar1=1.0,
                                                     scalar2=None, op0=ALU.add), [B_ada], [B_par])
            P.op("vector", lambda e: e.scalar_tensor_tensor(out=par[:, A2, :], in0=ada[:, 64:80], scalar=1.0,
                                                            in1=pv[:, 1, :], op0=ALU.add, op1=ALU.mult),
                 [B_ada, B_const], [B_par])
            P.op("vector", lambda e: e.tensor_copy(out=par[:, SH2, :], in_=ada[:, 48:64]), [B_ada], [B_par])
            P.op("vector", lambda e: e.tensor_scalar(out=par[:, G2C, :], in0=ada[:, 80:96], scalar1=1.0,
                                                     scalar2=None, op0=ALU.add), [B_ada], [B_par])
            P.op("vector", lambda e: e.tensor_copy(out=par[:, PSC, :], in_=pv[:, 2, :]), [B_const], [B_par])
            P.op("vector", lambda e: e.tensor_copy(out=par[:, HGN, :], in_=pv[:, 3, :]), [B_const], [B_par])
            B_tc = P.buf("tmpc")
            P.op("vector", lambda e: e.tensor_tensor(out=tmpc[:], in0=pv[:, 4, :], in1=pv[:, 5, :], op=ALU.subtract),
                 [B_const], [B_tc])
            P.op("scalar", lambda e: e.activation(out=par[:, LB, :], in_=tmpc[:], func=AF.Sigmoid), [B_tc], [B_par])
            P.op("vector", lambda e: e.tensor_scalar(out=par[:, OML, :], in0=par[:, LB, :], scalar1=-1.0, scalar2=1.0,
                                                     op0=ALU.mult, op1=ALU.add), [B_par], [B_par])
            B_gs = P.buf("gs")
            P.dma(lambda e: e.dma_start(out=g1s_d.rearrange("(j p) -> p j", p=128), in_=par[:, G1C, :],
                                        allow_slow_non_contiguous=True), "d_gs", reads=[B_par], writes=[B_gs])
            P.dma(lambda e: e.dma_start(out=g2s_d.rearrange("(j p) -> p j", p=128), in_=par[:, G2C, :],
                                        allow_slow_non_contiguous=True), "d_gs", reads=[B_par], writes=[B_gs])
            outs = [B_gs]
            if debug == "p0":
                B_dbg = P.buf("dbg")
                P.dma(lambda e: e.dma_start(out=dbg_d[:, 0:160], in_=par[:, 0:10, :].rearrange("p a b -> p (a b)")),
                      "d_dbg", reads=[B_par], writes=[B_dbg])
                outs.append(B_dbg)
            P.wait_all("sync", outs)
            P.fence()
        if debug == "p0":
            P.emit("main")
            return nc

        with ExitStack() as s1:
            sb = mk_alloc(s1)
            xt = sb("xt", [128, NTT, D])
            hT = sb("hT", [128, 16, TB], BF16)
            mT = sb("mT", [128, 16, TB], BF16)
            pe = sb("pe", [128, 8, 16 + TB])
            pooledT = sb("pooledT", [128, 8, TB], BF16)
            Sst = sb("Sst", [128, 16, 128])
            g1bc = sb("g1bc", [128, D])
            NW = 7
            PF = 5
            wbf = [sb(f"wbf{i}", [128, 16, 128], BF16) for i in range(NW)]
            poolw_b = sb("poolw_b", [128, 8, 512], BF16)
            psT = ps[3][:].bitcast(BF16)
            W = {}
            WB = {}
            alias = {"sig": 0, "f": 0, "lf": 1, "kk": 2, "cum": 3, "cumref": 4, "eq": 5, "ek": 6,
                     "sd": 7, "rstd": 7, "t1": 8, "t2": 8, "t3": 8, "u1": 9}
            Tt = [sb(f"wT{i}", [128, TB]) for i in range(10)]
            TBf = [P.buf(f"wT{i}") for i in range(10)]
            G3 = [[sb(f"g3_{i}_{j}", [128, TB], BF16) for j in range(3)] for i in range(2)]
            B_G3 = [[P.buf(f"g3_{i}_{j}") for j in range(3)] for i in range(2)]
            qr = sb("qr", [128, TB])
            B_qr = P.buf("qr")
            for n, i in alias.items():
                W[n] = Tt[i]
                WB[n] = TBf[i]
            qtb = sb("qtb", [128, TB], BF16)
            ktb = sb("ktb", [128, TB], BF16)
            iTb = sb("iTb", [128, TB], BF16)
            osqb = sb("osqb", [128, TB], BF16)
            tok = sb("tok", [128, 8, 128], BF16)
            attm = [sb(f"attm{i}", [128, 128], BF16) for i in range(4)]
            Sp = [sb(f"Sp{i}", [128, 128], BF16) for i in range(2)]
            tmpKI = [sb(f"tmpKI{i}", [128, 128]) for i in range(4)]
            esc = sb("esc", [128, 12])
            xnb = [sb(f"xn{i}", [128, D]) for i in range(2)]
            xn = xnb[0]
            statb = [sb(f"stat{i}", [128, 4]) for i in range(2)]
            pA = [sb(f"pA{i}", [128, 16 + TB]) for i in range(2)]
            o2t = sb("o2t", [128, NTT, 128])
            B_qtb, B_ktb, B_iTb, B_osqb, B_tok, B_esc, B_xn, B_stat, B_o2t = (
                P.buf(n) for n in ("qtb", "ktb", "iTb", "osqb", "tok", "esc", "xn", "stat", "o2t"))
            B_attm = P.bufs(4, "attm")
            B_Sp = P.bufs(2, "Sp")
            B_tmpKI = P.bufs(4, "tmpKI")
            B_pA = P.bufs(2, "pA")
            B_xt = P.bufs(NTT, "xt")
            B_hT = P.buf("hT")
            B_mT = P.buf("mT")
            B_pe = P.bufs(8, "pe")
            B_pooledT = P.buf("pooledT")
            B_S = P.bufs(16, "S")
            B_g1bc = P.buf("g1bc")
            B_wbf = P.bufs(NW, "wbf")
            B_poolw = P.buf("poolw")
            B_x1s = P.buf("x1s")
            wctr = [0]
            pctr = [0]

            blk_srcs = ([("win", c) for c in range(8)]
                        + [("win", base + hd) for hd in range(16) for base in (24, 88, 72, 56, 8, 40)]
                        + [("wout", n) for n in range(16)])
            all_srcs = blk_srcs * NBLK
            wiss = [0]

            def wstream(kind, idx):
                i = wctr[0]
                assert all_srcs[i] == (kind, idx), (i, all_srcs[i], kind, idx)
                while wiss[0] < min(len(all_srcs), i + 1 + PF):
                    j = wiss[0]
                    wiss[0] += 1
                    sj = j % NW
                    kd, ix = all_srcs[j]
                    src = win_d[ix] if kd == "win" else wout_d[ix]
                    P.dma((lambda sj, src: (lambda e: e.dma_start(out=wbf[sj][:], in_=src)))(sj, src), f"d_wbf{sj}",
                          writes=[B_wbf[sj]], eng="gpsimd")
                wctr[0] += 1
                s = i % NW
                return wbf[s], B_wbf[s]

            def proj(kind, idx, act_T, B_act):
                wb, wB = wstream(kind, idx)
                k = pctr[0] % 3
                pctr[0] += 1
                mm_group(P, ps[k][:, :], [(wb[:, kc, :], act_T[:, kc, :]) for kc in range(16)],
                         [wB, B_act], [psB[k]])
                return ps[k], psB[k]

            B_xnb = [B_xn, P.buf("xn1")]
            B_statb = [B_stat, P.buf("stat1")]

            def rms_to_T(tt, src, B_src, a_idx, sh_idx, dstT, B_dst):
                xn, B_xn, stat, B_stat = xnb[tt % 2], B_xnb[tt % 2], statb[tt % 2], B_statb[tt % 2]
                P.op("scalar", lambda e: e.activation(out=xn[:], in_=src, func=AF.Square), [B_src], [B_xn])
                P.op("vector", lambda e: e.tensor_reduce(out=stat[:, 0:1], in_=xn[:], axis=AX.X, op=ALU.add),
                     [B_xn], [B_stat])
                P.op("scalar", lambda e: e.activation(out=stat[:, 1:2], in_=stat[:, 0:1], func=AF.Ln,
                                                      scale=1.0 / D, bias=epsc[:, 0:1]), [B_stat, B_const], [B_stat])
                P.op("scalar", lambda e: e.activation(out=stat[:, 2:3], in_=stat[:, 1:2], func=AF.Exp, scale=-0.5),
                     [B_stat], [B_stat])
                P.op("vector", lambda e: e.tensor_scalar(out=xn[:], in0=src, scalar1=stat[:, 2:3], scalar2=None,
                                                         op0=ALU.mult), [B_src, B_stat], [B_xn])
                for gq in range(4):
                    k = pctr[0] % 3
                    pctr[0] += 1

                    def tfn(e, gq=gq, k=k):
                        ins = None
                        for j in range(4):
                            dc = gq * 4 + j
                            ins = e.transpose(out=ps[k][:, j * 128:(j + 1) * 128], in_=xn[:, dc * 128:(dc + 1) * 128],
                                              identity=identf[:])
                        return ins
                    P.op("tensor", tfn, [B_xn, B_const], [psB[k]])
                    for j in range(4):
                        dc = gq * 4 + j
                        o_ap = dstT[:, dc, tt * 128:(tt + 1) * 128]
                        i_ap = ps[k][:, j * 128:(j + 1) * 128]
                        if j % 2 == 0:
                            P.op("scalar", (lambda o_ap, i_ap, dc: (lambda e: e.activation(
                                out=o_ap, in_=i_ap, func=AF.Identity, scale=par[:, a_idx, dc:dc + 1],
                                bias=par[:, sh_idx, dc:dc + 1])))(o_ap, i_ap, dc), [psB[k], B_par], [B_dst])
                        else:
                            P.op("vector", (lambda o_ap, i_ap, dc: (lambda e: e.tensor_scalar(
                                out=o_ap, in0=i_ap, scalar1=par[:, a_idx, dc:dc + 1], scalar2=par[:, sh_idx, dc:dc + 1],
                                op0=ALU.mult, op1=ALU.add)))(o_ap, i_ap, dc), [psB[k], B_par], [B_dst])

            with ExitStack() as s1p:
                sbp = mk_alloc(s1p)
                xn4 = xn[:].rearrange("p (a b) -> p a b", a=4)
                for hf in range(2):
                    P.dma((lambda hf: (lambda e: e.dma_start(out=xn4, in_=poolw_d[:, hf * 4:hf * 4 + 4, :])))(hf), "d_pw",
                          writes=[B_xn])
                    P.op("vector", (lambda hf: (lambda e: e.tensor_copy(out=poolw_b[:, hf * 4:hf * 4 + 4, :], in_=xn4)))(hf),
                         [B_xn], [B_poolw])
                P.dma(lambda e: e.dma_start(out=g1bc[:], in_=g1s_d.partition_broadcast(128)), "d_g1bc",
                      writes=[B_g1bc])
                P.op("gpsimd", lambda e: e.memset(Sst[:], 0.0), [], B_S)
                P.op("gpsimd", lambda e: e.memset(pe[:], 0.0), [], B_pe)
                for i in range(2):
                    P.op("gpsimd", (lambda i: (lambda e: e.memset(pA[i][:], 0.0)))(i), [], [B_pA[i]])
                for i in range(4):
                    P.op("gpsimd", (lambda i: (lambda e: e.memset(attm[i][:], 0.0)))(i), [], [B_attm[i]])
                P.fence()

            for blk in range(NBLK):
                t0 = blk * TB
                for tt in range(NTT):
                    P.dma((lambda tt, t0: (lambda e: e.dma_start(out=xt[:, tt, :],
                                                                 in_=x_d[t0 + tt * 128:t0 + (tt + 1) * 128, :])))(tt, t0),
                          f"d_xt{tt}", writes=[B_xt[tt]])
                for tt in range(NTT):
                    rms_to_T(tt, xt[:, tt, :], B_xt[tt], A1, SH1, hT, B_hT)
                for c in range(8):
                    pp, pB = proj("win", c, hT, B_hT)
                    P.op("scalar", (lambda c, pp: (lambda e: e.copy(out=pe[:, c, 16:16 + TB], in_=pp[:, :])))(c, pp),
                         [pB], [B_pe[c]])
                    g = c // 2
                    w = (2, 4, 8, 16)[g]
                    cur = pe[:, c, :]
                    curB = B_pe[c]
                    sh = 1
                    i = 0
                    while sh < w:
                        dst = pA[i % 2]
                        dB = B_pA[i % 2]
                        P.op("gpsimd", (lambda dst, cur, sh: (lambda e: e.tensor_tensor(
                            out=dst[:, sh:16 + TB], in0=cur[:, sh:16 + TB], in1=cur[:, 0:16 + TB - sh], op=ALU.add)))(
                            dst, cur, sh), [curB], [dB])
                        cur = dst
                        curB = dB
                        sh *= 2
                        i += 1
                    P.op("vector", (lambda c, cur, w: (lambda e: e.scalar_tensor_tensor(
                        out=pooledT[:, c, :], in0=cur[:, 16:16 + TB], scalar=1.0 / w, in1=pe[:, c, 16:16 + TB],
                        op0=ALU.mult, op1=ALU.subtract)))(c, cur, w), [curB, B_pe[c]], [B_pooledT])
                    if blk == 0:
                        P.op("vector", (lambda c, cur, g: (lambda e: e.tensor_tensor(
                            out=cur[:, 16:32], in0=cur[:, 16:32], in1=pinv[:, g, :], op=ALU.mult)))(c, cur, g),
                            [curB, B_const], [curB])
                        P.op("vector", (lambda c, cur: (lambda e: e.tensor_tensor(
                            out=pooledT[:, c, 0:16], in0=cur[:, 16:32], in1=pe[:, c, 16:32], op=ALU.subtract)))(c, cur),
                            [curB, B_pe[c]], [B_pooledT])
                    P.op("gpsimd", (lambda c: (lambda e: e.tensor_copy(out=pe[:, c, 0:16], in_=pe[:, c, TB:TB + 16])))(c),
                         [B_pe[c]], [B_pe[c]])
                for hd in range(16):
                    par2 = hd % 2
                    sg, sbg, sa = G3[par2]
                    B_sg, B_sbg, B_sa = B_G3[par2]
                    def early(hd1, j):
                        tgt, B_tgt = ((W["sig"], WB["sig"]), (G3[hd1 % 2][1], B_G3[hd1 % 2][1]),
                                      (G3[hd1 % 2][2], B_G3[hd1 % 2][2]))[j]
                        pp, pB = proj("win", (24, 88, 72)[j] + hd1, hT, B_hT)
                        P.op("scalar", (lambda pp, tgt: (lambda e: e.activation(out=tgt[:], in_=pp[:, :],
                                                                                func=AF.Sigmoid)))(pp, tgt), [pB], [B_tgt])
                    if hd == 0:
                        for j in range(3):
                            early(0, j)
                    gp, gB = proj("win", 56 + hd, hT, B_hT)
                    P.op("scalar", (lambda gp, sg: (lambda e: e.activation(out=sg[:], in_=gp[:, :], func=AF.Silu)))(gp, sg),
                         [gB], [B_sg])
                    P.op("vector", (lambda hd: (lambda e: e.tensor_scalar(
                        out=W["f"][:], in0=W["sig"][:], scalar1=par[:, OML, hd:hd + 1], scalar2=par[:, LB, hd:hd + 1],
                        op0=ALU.mult, op1=ALU.add)))(hd), [WB["sig"], B_par], [WB["f"]])
                    P.op("scalar", lambda e: e.activation(out=W["lf"][:], in_=W["f"][:], func=AF.Ln),
                         [WB["f"]], [WB["lf"]])
                    P.op("gpsimd", lambda e: e.tensor_scalar(out=W["kk"][:], in0=W["f"][:], scalar1=-1.0, scalar2=1.0,
                                                             op0=ALU.mult, op1=ALU.add), [WB["f"]], [WB["kk"]])
                    for c in range(NTT):
                        P.op("vector", (lambda c: (lambda e: e.tensor_tensor_scan(
                            out=W["cum"][:, c * 128:(c + 1) * 128], data0=onesf[:], data1=W["lf"][:, c * 128:(c + 1) * 128],
                            initial=0.0, op0=ALU.mult, op1=ALU.add)))(c), [WB["lf"], B_const], [WB["cum"]])
                    qp, qB = proj("win", 8 + hd, hT, B_hT)
                    P.op("scalar", (lambda qp: (lambda e: e.copy(out=qr[:], in_=qp[:, :])))(qp), [qB], [B_qr])
                    ip, iB = proj("win", 40 + hd, hT, B_hT)
                    P.op("scalar", (lambda ip: (lambda e: e.copy(out=iTb[:], in_=ip[:, :])))(ip), [iB], [B_iTb])
                    cum3 = W["cum"][:].rearrange("p (c t) -> p c t", c=NTT)
                    cr3 = W["cumref"][:].rearrange("p (c t) -> p c t", c=NTT)
                    P.op("gpsimd", lambda e: e.tensor_tensor(out=cr3, in0=cum3,
                                                             in1=cum3[:, :, 63:64].to_broadcast([128, NTT, 128]),
                                                             op=ALU.subtract), [WB["cum"]], [WB["cumref"]])
                    P.op("scalar", lambda e: e.activation(out=W["eq"][:], in_=W["cumref"][:], func=AF.Exp),
                         [WB["cumref"]], [WB["eq"]])
                    P.op("scalar", lambda e: e.activation(out=W["ek"][:], in_=W["cumref"][:], func=AF.Exp, scale=-1.0),
                         [WB["cumref"]], [WB["ek"]])
                    P.op("scalar", lambda e: e.activation(out=esc[:, 0:4], in_=cum3[:, :, 63], func=AF.Exp),
                         [WB["cum"]], [B_esc])
                    P.op("scalar", lambda e: e.activation(out=esc[:, 4:8], in_=cum3[:, :, 127], func=AF.Exp),
                         [WB["cum"]], [B_esc])
                    P.op("scalar", lambda e: e.activation(out=esc[:, 8:12], in_=cr3[:, :, 127], func=AF.Exp),
                         [WB["cumref"]], [B_esc])
                    P.op("gpsimd", lambda e: e.tensor_tensor(out=ktb[:], in0=W["kk"][:], in1=W["ek"][:], op=ALU.mult),
                         [WB["kk"], WB["ek"]], [B_ktb])
                    P.op("vector", lambda e: e.tensor_tensor(out=qtb[:], in0=qr[:], in1=W["eq"][:], op=ALU.mult),
                         [B_qr, WB["eq"]], [B_qtb])

                    def tfn(e):
                        ins = None
                        for c in range(NTT):
                            ins = e.transpose(out=psT[:, c * 128:(c + 1) * 128], in_=iTb[:, c * 128:(c + 1) * 128],
                                              identity=identb[:])
                        for c in range(NTT):
                            ins = e.transpose(out=psT[:, (4 + c) * 128:(5 + c) * 128], in_=ktb[:, c * 128:(c + 1) * 128],
                                              identity=identb[:])
                        return ins
                    P.op("tensor", tfn, [B_iTb, B_ktb, B_const], [psB[3]])
                    P.op("vector", lambda e: e.tensor_copy(out=tok[:].rearrange("p a b -> p (a b)"), in_=psT[:, :]),
                         [psB[3]], [B_tok])
                    g = hd // 4
                    e0 = (hd % 4) * 128
                    kya = pctr[0] % 3
                    pctr[0] += 1
                    mm_group(P, ps[kya][:, :], [(poolw_b[:, g * 2 + kc, e0:e0 + 128], pooledT[:, g * 2 + kc, :]) for kc in range(2)],
                             [B_poolw, B_pooledT], [psB[kya]])
                    P.op("vector", (lambda hd, kya, sa: (lambda e: e.scalar_tensor_tensor(
                        out=W["u1"][:], in0=ps[kya][:, :], scalar=par[:, PSC, hd:hd + 1], in1=sa[:],
                        op0=ALU.mult, op1=ALU.mult)))(hd, kya, sa), [psB[kya], B_sa, B_par], [WB["u1"]])
                    for c in range(NTT):
                        def afn(e, c=c):
                            b0 = c * 128
                            e.matmul(psyb(0)[:, b0 + 64:b0 + 128], lhsT=ktb[:, b0:b0 + 128], rhs=qtb[:, b0 + 64:b0 + 128],
                                     start=True, stop=True)
                            return e.matmul(psyb(0)[0:64, b0:b0 + 64], lhsT=ktb[:, b0:b0 + 64], rhs=qtb[:, b0:b0 + 64],
                                            start=True, stop=True)
                        P.op("tensor", afn, [B_ktb, B_qtb], [psyB[0]])
                        P.op("vector", (lambda c: (lambda e: e.tensor_tensor(
                            out=attm[c][:, 64:128], in0=psyb(0)[:, c * 128 + 64:c * 128 + 128], in1=utm[:, 64:128],
                            op=ALU.mult)))(c), [psyB[0], B_const], [B_attm[c]])
                        P.op("vector", (lambda c: (lambda e: e.tensor_tensor(
                            out=attm[c][0:64, 0:64], in0=psyb(0)[0:64, c * 128:c * 128 + 64], in1=utm[0:64, 0:64],
                            op=ALU.mult)))(c), [psyB[0], B_const], [B_attm[c]])
                    for c in range(NTT):
                        cs_ = slice(c * 128, (c + 1) * 128)
                        P.op("tensor", (lambda c, cs_: (lambda e: e.matmul(psyb(2)[:, cs_], lhsT=tok[:, 4 + c, :],
                                                                           rhs=tok[:, c, :], start=True, stop=True)))(c, cs_),
                             [B_tok], [psyB[2]])
                        P.op("vector", (lambda c, cs_: (lambda e: e.tensor_scalar(
                            out=tmpKI[c][:], in0=psyb(2)[:, cs_], scalar1=esc[:, 8 + c:9 + c], scalar2=None,
                            op0=ALU.mult)))(c, cs_), [psyB[2], B_esc], [B_tmpKI[c]])
                    for c in range(NTT):
                        cs_ = slice(c * 128, (c + 1) * 128)
                        P.op("vector", (lambda c, hd: (lambda e: e.tensor_scalar(
                            out=Sp[c % 2][:], in0=Sst[:, hd, :], scalar1=esc[:, c:c + 1], scalar2=None, op0=ALU.mult)))(c, hd),
                            [B_S[hd], B_esc], [B_Sp[c % 2]])

                        def ofn(e, c=c, cs_=cs_):
                            e.matmul(psyb(1)[:, cs_], lhsT=tok[:, c, :], rhs=attm[c][:], start=True, stop=False)
                            return e.matmul(psyb(1)[:, cs_], lhsT=Sp[c % 2][:], rhs=qtb[:, cs_], start=False, stop=True)
                        P.op("tensor", ofn, [B_tok, B_attm[c], B_Sp[c % 2], B_qtb], [psyB[1]])
                        if c < 3 and hd + 1 < 16:
                            early(hd + 1, c)
                        if not (blk == NBLK - 1 and c == NTT - 1):
                            P.op("vector", (lambda c, hd: (lambda e: e.scalar_tensor_tensor(
                                out=Sst[:, hd, :], in0=Sst[:, hd, :], scalar=esc[:, 4 + c:5 + c], in1=tmpKI[c][:],
                                op0=ALU.mult, op1=ALU.add)))(c, hd), [B_S[hd], B_esc, B_tmpKI[c]], [B_S[hd]])
                    P.op("scalar", lambda e: e.activation(out=osqb[:], in_=psyb(1), func=AF.Square), [psyB[1]], [B_osqb])
                    P.op("tensor", lambda e: e.matmul(psyb(3), lhsT=onesb[:], rhs=osqb[:], start=True, stop=True),
                         [B_osqb, B_const], [psyB[3]])
                    P.op("scalar", lambda e: e.activation(out=W["sd"][:], in_=psyb(3), func=AF.Ln, scale=1.0 / 128,
                                                          bias=epsc[:, 0:1]), [psyB[3], B_const], [WB["sd"]])
                    P.op("scalar", lambda e: e.activation(out=W["rstd"][:], in_=W["sd"][:], func=AF.Exp, scale=-0.5),
                         [WB["sd"]], [WB["rstd"]])
                    P.op("vector", lambda e: e.tensor_tensor(out=W["t1"][:], in0=psyb(1), in1=W["rstd"][:], op=ALU.mult),
                         [psyB[1], WB["rstd"]], [WB["t1"]])
                    P.op("vector", (lambda hd, sg: (lambda e: e.scalar_tensor_tensor(
                        out=W["t2"][:], in0=W["t1"][:], scalar=par[:, HGN, hd:hd + 1], in1=sg[:],
                        op0=ALU.mult, op1=ALU.mult)))(hd, sg), [WB["t1"], B_sg, B_par], [WB["t2"]])
                    P.op("gpsimd", (lambda sbg: (lambda e: e.tensor_tensor(out=W["t3"][:], in0=W["t2"][:], in1=sbg[:],
                                                                           op=ALU.mult)))(sbg),
                         [WB["t2"], B_sbg], [WB["t3"]])
                    P.op("gpsimd", (lambda hd: (lambda e: e.tensor_tensor(out=mT[:, hd, :], in0=W["u1"][:], in1=W["t3"][:],
                                                                          op=ALU.add)))(hd), [WB["u1"], WB["t3"]], [B_mT])
                for nch in range(16):
                    wb, wB = wstream("wout", nch)
                    k = pctr[0] % 3
                    pctr[0] += 1

                    def wfn(e, wb=wb, k=k):
                        ins = None
                        for tt in range(NTT):
                            for fc in range(16):
                                ins = e.matmul(ps[k][:, tt * 128:(tt + 1) * 128], lhsT=mT[:, fc, tt * 128:(tt + 1) * 128],
                                               rhs=wb[:, fc, :], start=(fc == 0), stop=(fc == 15))
                        return ins
                    P.op("tensor", wfn, [wB, B_mT], [psB[k]])
                    P.op("vector", (lambda k, nch: (lambda e: e.tensor_tensor(
                        out=o2t[:], in0=ps[k][:, :].rearrange("p (a b) -> p a b", a=NTT),
                        in1=g1bc[:, nch * 128:(nch + 1) * 128].unsqueeze(1).to_broadcast([128, NTT, 128]),
                        op=ALU.mult)))(k, nch), [psB[k], B_g1bc], [B_o2t])
                    P.op("gpsimd", (lambda nch: (lambda e: e.tensor_tensor(
                        out=xt[:, :, nch * 128:(nch + 1) * 128], in0=xt[:, :, nch * 128:(nch + 1) * 128], in1=o2t[:],
                        op=ALU.add)))(nch), [B_o2t] + B_xt, B_xt)
                for tt in range(NTT):
                    P.dma((lambda tt, t0: (lambda e: e.dma_start(out=x1s_d[t0 + tt * 128:t0 + (tt + 1) * 128, :],
                                                                 in_=xt[:, tt, :])))(tt, t0), "d_x1s", reads=[B_xt[tt]],
                          writes=[B_x1s])
                P.wait_all("sync", [B_x1s])
                P.fence()
        if debug == "p1":
            P.emit("main")
            return nc

        THR = 1.0 - 2.0e-6
        with ExitStack() as s2:
            sb = mk_alloc(s2)
            hT = sb("h2T", [128, 16, TB], BF16)
            qM = sb("qM", [128, 16 * TB], BF16)
            qT = qM[:].rearrange("p (g t) -> p g t", g=16)
            Mb1 = sb("Mb1", [128, 16 * TB], BF16)
            Mv = [qM[:].rearrange("p (tt h a b) -> p tt h a b", tt=NTT, h=8, a=2),
                  Mb1[:].rearrange("p (tt h a b) -> p tt h a b", tt=NTT, h=8, a=2)]
            E1b = sb("E1b", [128, NTT, 8, 128], BF16)
            E0p = sb("E0p", [128, NTT, 8, 128])
            Dm = sb("Dm", [128, NTT, 8, 128], BF16)
            yacc = sb("yacc", [128, NTT, D])
            with ExitStack() as sA:
                sba = mk_alloc(sA)
                NWQ = 4
                a_wbf = [sba(f"wbf{i}", [128, 16, 128], BF16) for i in range(NWQ)]
                a_xtmp = [sba(f"xtmp{i}", [128, D]) for i in range(2)]
                a_xn = [sba(f"xn{i}", [128, D]) for i in range(2)]
                a_stat = [sba(f"stat{i}", [128, 4]) for i in range(2)]
                a_Ef = sba("Ef", [128, 16, 128])
                a_Ebt = sba("Ebt", [128, 16, 128], BF16)
                a_wk = sba("wk", [128, 16, 128])
                a_top = sba("top", [128, 16, 16])
                a_cand = sba("cand", [128, 8, 256])
                a_ctop = sba("ctop", [128, 8, 16])
                a_smax = sba("smax", [128, 16])
                a_zz = sba("zz", [128, 32])
                a_kst = sba("kst", [128, 16, 128])
                a_keysb = sba("keysb", [128, 16, 128], BF16)
            with ExitStack() as sM:
                sbm = mk_alloc(sM)
                m_utb = [sbm(f"utb{i}", [128, 16, 128], BF16) for i in range(6)]
                m_vb = [sbm(f"vb{i}", [128, D], BF16) for i in range(8)]
                m_ge = [sbm(f"ge{i}", [128, TB], BF16) for i in range(2)]
                m_AT = [sbm(f"AT{i}", [128, TB], BF16) for i in range(8)]
                m_Pf = [sbm(f"Pf{i}", [128, 8, 2, 128]) for i in range(3)]
            with ExitStack() as sE:
                sbe = mk_alloc(sE)
                e_xtmp = [sbe(f"xe{i}", [128, D]) for i in range(4)]
                e_g2bc = sbe("g2bc", [128, D])
                e_fnbc = sbe("fnbc", [128, D])
                e_junk = [sbe(f"junk{i}", [128, D]) for i in range(4)]
                e_stat = [sbe(f"state{i}", [128, 4]) for i in range(4)]

            def peer_prologue(blk):
                t0 = blk * TB
                wbf, xtmp, xn, stat, Ef, Ebt, wk, top, cand, ctop, smax, zz, kst, keysb = (
                    a_wbf, a_xtmp, a_xn, a_stat, a_Ef, a_Ebt, a_wk, a_top, a_cand, a_ctop, a_smax, a_zz, a_kst,
                    a_keysb)
                B_wbf = P.bufs(NWQ, "wbf2")
                B_xtmp = P.bufs(2, "xtmp")
                B_xn, B_stat, B_Ef, B_Ebt, B_wk, B_top, B_cand, B_ctop, B_smax, B_zz, B_kst, B_keys = (
                    P.buf(n) for n in ("xn2", "stat2", "Ef", "Ebt", "wk", "top", "cand", "ctop", "smax", "zz", "kst",
                                       "keysb"))
                B_hT = P.buf("h2T")
                B_qT = P.buf("qT")
                B_E1 = P.bufs(NTT, "E1b")
                B_E0 = P.bufs(NTT, "E0p")
                B_Dm = P.bufs(NTT, "Dm")
                P.dma(lambda e: e.dma_start(out=kst[:], in_=keysT_d), "d_kst", writes=[B_kst])
                P.op("vector", lambda e: e.tensor_copy(out=keysb[:], in_=kst[:]), [B_kst], [B_keys])
                pctr = [0]
                B_xn2 = [B_xn, P.buf("xn2b")]
                B_stat2 = [B_stat, P.buf("stat2b")]

                def norm_tt(tt, xn, stat, B_xn, B_stat):
                        P.dma((lambda tt: (lambda e: e.dma_start(out=xtmp[tt % 2][:],
                                                                 in_=x1s_d[t0 + tt * 128:t0 + (tt + 1) * 128, :])))(tt),
                              f"d_xtmp{tt % 2}", writes=[B_xtmp[tt % 2]])
                        src = xtmp[tt % 2][:]
                        B_src = B_xtmp[tt % 2]
                        P.op("scalar", (lambda src: (lambda e: e.activation(out=xn[:], in_=src, func=AF.Square)))(src),
                             [B_src], [B_xn])
                        P.op("vector", lambda e: e.tensor_reduce(out=stat[:, 0:1], in_=xn[:], axis=AX.X, op=ALU.add),
                             [B_xn], [B_stat])
                        P.op("scalar", lambda e: e.activation(out=stat[:, 1:2], in_=stat[:, 0:1], func=AF.Ln,
                                                              scale=1.0 / D, bias=epsc[:, 0:1]), [B_stat, B_const], [B_stat])
                        P.op("scalar", lambda e: e.activation(out=stat[:, 2:3], in_=stat[:, 1:2], func=AF.Exp, scale=-0.5),
                             [B_stat], [B_stat])
                        P.op("vector", (lambda src: (lambda e: e.tensor_scalar(out=xn[:], in0=src, scalar1=stat[:, 2:3],
                                                                               scalar2=None, op0=ALU.mult)))(src),
                             [B_src, B_stat], [B_xn])
                        for gq in range(4):
                            k = pctr[0] % 3
                            pctr[0] += 1

                            def tfn(e, gq=gq, k=k):
                                ins = None
                                for j in range(4):
                                    dc = gq * 4 + j
                                    ins = e.transpose(out=ps[k][:, j * 128:(j + 1) * 128],
                                                      in_=xn[:, dc * 128:(dc + 1) * 128], identity=identf[:])
                                return ins
                            P.op("tensor", tfn, [B_xn, B_const], [psB[k]])
                            for j in range(4):
                                dc = gq * 4 + j
                                o_ap = hT[:, dc, tt * 128:(tt + 1) * 128]
                                i_ap = ps[k][:, j * 128:(j + 1) * 128]
                                if j % 2 == 0:
                                    P.op("scalar", (lambda o_ap, i_ap, dc: (lambda e: e.activation(
                                        out=o_ap, in_=i_ap, func=AF.Identity, scale=par[:, A2, dc:dc + 1],
                                        bias=par[:, SH2, dc:dc + 1])))(o_ap, i_ap, dc), [psB[k], B_par], [B_hT])
                                else:
                                    P.op("vector", (lambda o_ap, i_ap, dc: (lambda e: e.tensor_scalar(
                                        out=o_ap, in0=i_ap, scalar1=par[:, A2, dc:dc + 1], scalar2=par[:, SH2, dc:dc + 1],
                                        op0=ALU.mult, op1=ALU.add)))(o_ap, i_ap, dc), [psB[k], B_par], [B_hT])

                for tt in range(NTT):
                    norm_tt(tt, xn[tt % 2], stat[tt % 2], B_xn2[tt % 2], B_stat2[tt % 2])
                for ch in range(16):
                    s = ch % NWQ
                    if ch == 0:
                        for c2 in range(min(3, 16)):
                            P.dma((lambda c2: (lambda e: e.dma_start(out=wbf[c2 % NWQ][:], in_=wq_d[c2])))(c2),
                                  f"d_wbfq{c2 % NWQ}", writes=[B_wbf[c2 % NWQ]], eng="gpsimd")
                    if ch + 3 < 16:
                        c2 = ch + 3
                        P.dma((lambda c2: (lambda e: e.dma_start(out=wbf[c2 % NWQ][:], in_=wq_d[c2])))(c2),
                              f"d_wbfq{c2 % NWQ}", writes=[B_wbf[c2 % NWQ]], eng="gpsimd")
                    k = pctr[0] % 3
                    pctr[0] += 1
                    mm_group(P, ps[k][:, :], [(wbf[s][:, kc, :], hT[:, kc, :]) for kc in range(16)],
                             [B_wbf[s], B_hT], [psB[k]])
                    P.op("scalar", (lambda ch, k: (lambda e: e.copy(out=qT[:, ch, :], in_=ps[k][:, :])))(ch, k),
                         [psB[k]], [B_qT])
                for tt in range(NTT):
                    for bk in range(4):
                        def sfn(e, tt=tt, bk=bk):
                            ins = None
                            for j in range(4):
                                g = bk * 4 + j
                                ins = e.matmul(psyb(bk)[:, j * 128:(j + 1) * 128], lhsT=qT[:, g, tt * 128:(tt + 1) * 128],
                                               rhs=keysb[:, g, :], start=True, stop=True)
                            return ins
                        P.op("tensor", sfn, [B_qT, B_keys], [psyB[bk]])
                        pv3 = psyb(bk).rearrange("p (a b) -> p a b", a=4)
                        P.op("vector", (lambda bk, pv3: (lambda e: e.tensor_reduce(
                            out=smax[:, bk * 4:bk * 4 + 4], in_=pv3, axis=AX.X, op=ALU.max)))(bk, pv3),
                            [psyB[bk]], [B_smax])
                        P.op("vector", (lambda bk, pv3: (lambda e: e.tensor_tensor(
                            out=Ef[:, bk * 4:bk * 4 + 4, :], in0=pv3,
                            in1=smax[:, bk * 4:bk * 4 + 4].unsqueeze(2).to_broadcast([128, 4, 128]),
                            op=ALU.subtract)))(bk, pv3), [psyB[bk], B_smax], [B_Ef])
                    P.op("scalar", lambda e: e.activation(out=Ebt[:], in_=Ef[:], func=AF.Exp), [B_Ef], [B_Ebt])
                    P.op("vector", lambda e: e.tensor_copy(out=Ef[:], in_=Ebt[:]), [B_Ebt], [B_Ef])
                    Eb4 = Ebt[:].rearrange("p (h two) n -> p h two n", two=2)
                    Ef4 = Ef[:].rearrange("p (h two) n -> p h two n", two=2)
                    P.op("gpsimd", (lambda tt, Eb4: (lambda e: e.tensor_copy(out=E1b[:, tt, :, :], in_=Eb4[:, :, 1, :])))(tt, Eb4),
                         [B_Ebt], [B_E1[tt]])
                    B_topg = P.bufs(16, "topg")
                    B_wkg = P.bufs(16, "wkg")
                    for g in range(16):
                        P.op("vector", (lambda g: (lambda e: e.max(out=top[:, g, 0:8], in_=Ef[:, g, :])))(g),
                             [B_Ef, B_top], [B_topg[g]])
                    for g in range(16):
                        P.op("vector", (lambda g: (lambda e: e.match_replace(
                            out=wk[:, g, :], in_to_replace=top[:, g, 0:8], in_values=Ef[:, g, :], imm_value=NEG)))(g),
                            [B_Ef, B_topg[g], B_wk], [B_wkg[g]])
                    for g in range(16):
                        P.op("vector", (lambda g: (lambda e: e.max(out=top[:, g, 8:16], in_=wk[:, g, :])))(g),
                             [B_wkg[g]], [B_topg[g]])
                    top4 = top[:].rearrange("p (h two) k -> p h two k", two=2)
                    P.op("vector", lambda e: e.tensor_tensor(
                        out=cand[:].rearrange("p h (i j) -> p h i j", i=16),
                        in0=top4[:, :, 0, :].unsqueeze(3).to_broadcast([128, 8, 16, 16]),
                        in1=top4[:, :, 1, :].unsqueeze(2).to_broadcast([128, 8, 16, 16]), op=ALU.mult),
                        B_topg, [B_cand, B_top])
                    wk8 = wk[:].rearrange("p (h a) b -> p h (a b)", a=2)
                    B_ctoph = P.bufs(8, "ctoph")
                    B_wkh = P.bufs(8, "wkh")
                    for h in range(8):
                        P.op("vector", (lambda h: (lambda e: e.max(out=ctop[:, h, 0:8], in_=cand[:, h, :])))(h),
                             [B_cand, B_ctop], [B_ctoph[h]])
                    for h in range(8):
                        P.op("vector", (lambda h, wk8: (lambda e: e.match_replace(
                            out=wk8[:, h, :], in_to_replace=ctop[:, h, 0:8], in_values=cand[:, h, :], imm_value=NEG)))(h, wk8),
                            [B_cand, B_ctoph[h]] + B_wkg, [B_wkh[h]])
                    for h in range(8):
                        P.op("vector", (lambda h, wk8: (lambda e: e.max(out=ctop[:, h, 8:16], in_=wk8[:, h, :])))(h, wk8),
                             [B_wkh[h]], [B_ctoph[h]])
                    B_ctop_all = B_ctoph
                    P.op("vector", lambda e: e.reciprocal(out=zz[:, 24:32], in_=ctop[:, :, 15]), B_ctop_all, [B_zz, B_ctop, B_wk])
                    P.op("vector", (lambda tt, Ef4: (lambda e: e.tensor_tensor(
                        out=E0p[:, tt, :, :], in0=Ef4[:, :, 0, :],
                        in1=zz[:, 24:32].unsqueeze(2).to_broadcast([128, 8, 128]), op=ALU.mult)))(tt, Ef4),
                        [B_Ef, B_zz], [B_E0[tt]])
                    P.op("vector", lambda e: e.tensor_reduce(out=zz[:, 0:8], in_=ctop[:], axis=AX.X, op=ALU.add),
                         B_ctop_all, [B_zz])
                    P.op("vector", lambda e: e.reciprocal(out=zz[:, 8:16], in_=zz[:, 0:8]), [B_zz], [B_zz])
                    P.op("vector", lambda e: e.tensor_tensor(out=zz[:, 16:24], in0=zz[:, 8:16], in1=ctop[:, :, 15],
                                                             op=ALU.mult), [B_zz] + B_ctop_all, [B_zz])
                    for h in range(8):
                        P.op("gpsimd", (lambda tt, h: (lambda e: e.tensor_scalar(
                            out=Dm[:, tt, h, :], in0=identf[:], scalar1=zz[:, 16 + h:17 + h], scalar2=None,
                            op0=ALU.mult)))(tt, h), [B_zz, B_const], [B_Dm[tt]])
                return B_hT, B_E1, B_E0, B_Dm

            def peer_main(blk, B_hT, B_E1, B_E0, B_Dm):
                utb, vb, ge, AT, Pf = m_utb, m_vb, m_ge, m_AT, m_Pf
                B_utb = P.bufs(6, "utb")
                B_vb = P.bufs(8, "vb")
                B_ge = P.bufs(2, "ge")
                B_AT = P.bufs(8, "AT")
                B_Pf = P.bufs(3, "Pf")
                B_M = [P.bufs(NTT, "M0_"), P.bufs(NTT, "M1_")]
                B_yacc = P.bufs(NTT, "yacc")
                pfc = [0]

                def stageA(sc):
                    mv = Mv[sc % 2]
                    for tt in range(NTT):
                        pf = Pf[pfc[0] % 3]
                        B_pf = B_Pf[pfc[0] % 3]
                        pfc[0] += 1
                        if tt % 2 == 0:
                            P.op("gpsimd", (lambda pf, tt, sc: (lambda e: e.tensor_tensor(
                                out=pf[:],
                                in0=E0p[:, tt, :, 2 * sc:2 * sc + 2].unsqueeze(3).to_broadcast([128, 8, 2, 128]),
                                in1=E1b[:, tt, :, :].unsqueeze(2).to_broadcast([128, 8, 2, 128]), op=ALU.mult)))(pf, tt, sc),
                                [B_E0[tt], B_E1[tt]], [B_pf])
                        else:
                            def pfn(e, pf=pf, tt=tt, sc=sc):
                                ins = None
                                for h in range(8):
                                    for a in range(2):
                                        ins = e.activation(out=pf[:, h, a, :], in_=E1b[:, tt, h, :], func=AF.Copy,
                                                           scale=E0p[:, tt, h, 2 * sc + a:2 * sc + a + 1])
                                return ins
                            P.op("scalar", pfn, [B_E0[tt], B_E1[tt]], [B_pf])
                        P.op("vector", (lambda pf, tt, mv: (lambda e: e.scalar_tensor_tensor(
                            out=mv[:, tt, :, :, :], in0=pf[:], scalar=THR, in1=pf[:],
                            op0=ALU.is_ge, op1=ALU.mult)))(pf, tt, mv), [B_pf], [B_M[sc % 2][tt]])

                def stageDU(sc):
                    for k in range(2):
                        et = 2 * sc + k
                        P.dma((lambda et: (lambda e: e.dma_start(out=utb[et % 6][:], in_=UT_d[et])))(et),
                              f"d_utb{et % 6}", writes=[B_utb[et % 6]], eng="gpsimd")

                def stageDV(sc):
                    for k in range(2):
                        et = 2 * sc + k
                        P.dma((lambda et: (lambda e: e.dma_start(out=vb[et % 8][:], in_=V_d[et])))(et),
                              f"d_vb{et % 8}", writes=[B_vb[et % 8]], eng="gpsimd")

                def stageB(sc):
                    mv = Mv[sc % 2]
                    for k in range(2):
                        et = 2 * sc + k
                        s4 = et % 8
                        u6 = et % 6
                        mm_group(P, ps[k][:, :], [(utb[u6][:, dc, :], hT[:, dc, :]) for dc in range(16)],
                                 [B_utb[u6], B_hT], [psB[k]])

                        def gfn(e, k=k, mv=mv):
                            ins = None
                            for tt in range(NTT):
                                for h in range(8):
                                    ins = e.matmul(ps[2 + k][:, tt * 128:(tt + 1) * 128], lhsT=mv[:, tt, h, k, :],
                                                   rhs=Dm[:, tt, h, :], start=(h == 0), stop=(h == 7))
                            return ins
                        P.op("tensor", gfn, B_M[sc % 2] + B_Dm, [psB[2 + k]])
                        P.op("scalar", (lambda k: (lambda e: e.activation(out=ge[k][:], in_=ps[k][:, :], func=AF.Gelu)))(k),
                             [psB[k]], [B_ge[k]])
                        P.op("vector", (lambda k, s4: (lambda e: e.tensor_tensor(out=AT[s4][:], in0=ps[2 + k][:, :],
                                                                                 in1=ge[k][:], op=ALU.mult)))(k, s4),
                             [psB[2 + k], B_ge[k]], [B_AT[s4]])

                def stageC(j):
                    et0 = 4 * j
                    sl = [(et0 + k) % 8 for k in range(4)]
                    for tt in range(NTT):
                        for hf in range(2):
                            def yfn(e, tt=tt, hf=hf, sl=sl):
                                ins = None
                                for n in (2 * hf, 2 * hf + 1):
                                    for k in range(4):
                                        ins = e.matmul(psyb(n), lhsT=AT[sl[k]][:, tt * 128:(tt + 1) * 128],
                                                       rhs=vb[sl[k]][:, n * 512:(n + 1) * 512], start=(k == 0),
                                                       stop=(k == 3))
                                return ins
                            pb = [psyB[2 * hf], psyB[2 * hf + 1]]
                            P.op("tensor", yfn, [B_AT[i] for i in sl] + [B_vb[i] for i in sl], pb)
                            cs_ = slice(hf * 1024, (hf + 1) * 1024)
                            if j == 0:
                                P.op("vector", (lambda tt, cs_: (lambda e: e.tensor_copy(out=yacc[:, tt, cs_],
                                                                                         in_=psy[:, cs_])))(tt, cs_),
                                     pb, [B_yacc[tt]])
                            else:
                                P.op("vector", (lambda tt, cs_: (lambda e: e.tensor_tensor(
                                    out=yacc[:, tt, cs_], in0=psy[:, cs_], in1=yacc[:, tt, cs_], op=ALU.add)))(tt, cs_),
                                    pb + [B_yacc[tt]], [B_yacc[tt]])

                NSC = 64
                stageDU(0)
                stageDU(1)
                stageDV(0)
                stageA(0)
                for sc in range(NSC):
                    if sc + 1 < NSC:
                        stageA(sc + 1)
                    stageB(sc)
                    if sc + 2 < NSC:
                        stageDU(sc + 2)
                    if sc + 1 < NSC:
                        stageDV(sc + 1)
                    if sc % 2 == 0 and sc >= 2:
                        stageC((sc - 2) // 2)
                stageC(NSC // 2 - 1)
                return B_yacc

            def peer_epilogue(blk, B_yacc):
                t0 = blk * TB
                xtmp, g2bc, fnbc = e_xtmp, e_g2bc, e_fnbc
                B_xe = P.bufs(4, "xe")
                B_bc = P.buf("bc2")
                B_out = P.buf("out")
                P.dma(lambda e: e.dma_start(out=g2bc[:], in_=g2s_d.partition_broadcast(128)), "d_bc2", writes=[B_bc],
                      eng="scalar")
                P.dma(lambda e: e.dma_start(out=fnbc[:], in_=fnorm_d.partition_broadcast(128)), "d_bc2", writes=[B_bc],
                      eng="scalar")

                def ep_load(tt, s):
                    P.dma(lambda e: e.dma_start(out=xtmp[s][:], in_=x1s_d[t0 + tt * 128:t0 + (tt + 1) * 128, :]),
                          f"d_xe{s}", writes=[B_xe[s]])
                for tt in range(NTT):
                    ep_load(tt, tt)

                B_junk4 = P.bufs(4, "junk")
                B_st4 = P.bufs(4, "state")

                def ep_stage(k, tt):
                    s = tt
                    junk, stat, B_junk, B_st = e_junk[tt], e_stat[tt], B_junk4[tt], B_st4[tt]
                    if k == 0:
                        P.op("gpsimd", lambda e: e.tensor_tensor(out=yacc[:, tt, :], in0=yacc[:, tt, :], in1=g2bc[:],
                                                                 op=ALU.mult), [B_yacc[tt], B_bc], [B_yacc[tt]])
                        P.op("vector", lambda e: e.tensor_tensor(out=xtmp[s][:], in0=xtmp[s][:], in1=yacc[:, tt, :],
                                                                 op=ALU.add), [B_xe[s], B_yacc[tt]], [B_xe[s]])
                        P.op("scalar", lambda e: e.activation(out=junk[:], in_=xtmp[s][:], func=AF.Square),
                             [B_xe[s]], [B_junk])
                    elif k == 1:
                        P.op("vector", lambda e: e.tensor_reduce(out=stat[:, 0:1], in_=junk[:], axis=AX.X, op=ALU.add),
                             [B_junk], [B_st])
                        P.op("scalar", lambda e: e.activation(out=stat[:, 1:2], in_=stat[:, 0:1], func=AF.Ln,
                                                              scale=1.0 / D, bias=epsc[:, 0:1]), [B_st, B_const], [B_st])
                        P.op("scalar", lambda e: e.activation(out=stat[:, 2:3], in_=stat[:, 1:2], func=AF.Exp, scale=-0.5),
                             [B_st], [B_st])
                    else:
                        P.op("vector", lambda e: e.scalar_tensor_tensor(
                            out=xtmp[s][:], in0=xtmp[s][:], scalar=stat[:, 2:3], in1=fnbc[:], op0=ALU.mult,
                            op1=ALU.mult), [B_xe[s], B_st, B_bc], [B_xe[s]])
                        P.dma(lambda e: e.dma_start(out=out_d[t0 + tt * 128:t0 + (tt + 1) * 128, :], in_=xtmp[s][:]),
                              f"d_out{s}", reads=[B_xe[s]], writes=[B_out])
                for k in range(3):
                    for tt in range(NTT):
                        ep_stage(k, tt)
                return B_out

            outs = []
            for blk in range(NBLK):
                P.fence()
                r = peer_prologue(blk)
                P.fence()
                if debug == "p2a":
                    dbg2 = nc.dram_tensor("dbg2", [128, NTT * 8 * 128 * 3], F32, kind="ExternalOutput").ap()
                    cv = sb("cv", [128, NTT * 8 * 128])
                    B_cv = P.buf("cv")
                    B_o = P.buf("dbgo")
                    n1 = NTT * 8 * 128
                    P.dma(lambda e: e.dma_start(out=dbg2[:, 0:n1], in_=E0p[:].rearrange("p a b c -> p (a b c)")), "d_dbg2",
                          reads=r[2], writes=[B_o])
                    P.op("vector", lambda e: e.tensor_copy(out=cv[:], in_=E1b[:].rearrange("p a b c -> p (a b c)")),
                         r[1], [B_cv])
                    P.dma(lambda e: e.dma_start(out=dbg2[:, n1:2 * n1], in_=cv[:]), "d_dbg2", reads=[B_cv], writes=[B_o])
                    cv2 = sb("cv2", [128, NTT * 8 * 128])
                    B_cv2 = P.buf("cv2")
                    P.op("vector", lambda e: e.tensor_copy(out=cv2[:], in_=Dm[:].rearrange("p a b c -> p (a b c)")),
                         r[3], [B_cv2])
                    P.dma(lambda e: e.dma_start(out=dbg2[:, 2 * n1:3 * n1], in_=cv2[:]), "d_dbg2", reads=[B_cv2], writes=[B_o])
                    P.wait_all("sync", [B_o])
                    P.emit("main")
                    return nc
                B_yacc = peer_main(blk, *r)
                P.fence()
                if debug == "p2m":
                    dbg3 = nc.dram_tensor("dbg3", [128, NTT * D], F32, kind="ExternalOutput").ap()
                    B_o = P.buf("dbgo")
                    P.dma(lambda e: e.dma_start(out=dbg3, in_=yacc[:].rearrange("p a b -> p (a b)")), "d_dbg3",
                          reads=B_yacc, writes=[B_o])
                    P.wait_all("sync", [B_o])
                    P.emit("main")
                    return nc
                outs.append(peer_epilogue(blk, B_yacc))
                if debug == "p2e" and blk == 1:
                    break
            P.wait_all("sync", outs)
        P.emit("main")
    return nc


def _tile_w(w):
    K, N = w.shape
    return np.ascontiguousarray(w.reshape(16, 128, N // 128, 128).transpose(2, 1, 0, 3))


def _cols(v):
    return np.ascontiguousarray(np.asarray(v, np.float32).reshape(-1, 128).T)


def prep_inputs(x, c, w_ada, b_ada, norm1, w_in, pool_w, pool_scale, lb_logits, hg_norm, w_out, norm2, peer_wq,
                peer_keys, peer_u, peer_v, final_norm):
    f = lambda a: np.asarray(a, np.float32)
    sh = {}
    wa = f(w_ada)[0]
    sh["wada"] = np.ascontiguousarray(wa.reshape(16, 128, 24, 512).transpose(2, 1, 0, 3))
    sh["bada"] = _cols(f(b_ada)[0])
    sh["pvec"] = np.ascontiguousarray(np.stack([_cols(f(norm1)[0]), _cols(f(norm2)[0]), _cols(f(pool_scale)[0]),
                                                _cols(f(hg_norm)[0].reshape(-1)), _cols(f(lb_logits)[0]),
                                                _cols(f(lb_logits)[1])], axis=1))
    sh["fnorm"] = np.ascontiguousarray(f(final_norm))
    sh["win"] = _tile_w(f(w_in)[0])
    sh["poolw"] = np.ascontiguousarray(f(pool_w)[0].reshape(4, 2, 128, 512).transpose(2, 0, 1, 3).reshape(128, 8, 512))
    sh["wout"] = _tile_w(f(w_out)[0])
    sh["wq"] = _tile_w(f(peer_wq)[0])
    sh["keysT"] = np.ascontiguousarray(f(peer_keys)[0].transpose(3, 0, 1, 2).reshape(128, 16, 128))
    sh["UT"] = np.ascontiguousarray(f(peer_u)[0].reshape(128, 128, 16, 128).transpose(0, 3, 2, 1))
    sh["V"] = np.ascontiguousarray(f(peer_v)[0].reshape(128, 128, D))
    sh["ident"] = np.eye(128, dtype=np.float32)
    sh["utm"] = np.triu(np.ones((128, 128), np.float32))
    pinv = np.zeros((128, 4, 16), np.float32)
    for g, w in enumerate((2, 4, 8, 16)):
        pinv[:, g, :] = 1.0 / np.minimum(np.arange(16) + 1, w).astype(np.float32)[None, :]
    sh["pinv"] = pinv
    xs = f(x)
    cs = f(c)
    maps = []
    for b in range(xs.shape[0]):
        m = dict(sh)
        m["x"] = np.ascontiguousarray(xs[b])
        m["cT"] = _cols(cs[b])
        maps.append(m)
    return maps


def kernel(**inputs):
    maps = prep_inputs(**inputs)
    nc = build_program()
    res = run_bass_kernel_spmd(nc, maps, core_ids=list(range(len(maps))))
    out = np.stack([np.asarray(r["out"], np.float32) for r in res.results], axis=0)
    return out
```
